# Optimizing a Trainium2 kernel written in Bass

```python
import math
import jax
import jax.numpy as jnp
from jax import lax
import numpy as np

D_MODEL = 1024
BATCH = 16
SEQ = 2048
DEPTH = 1

BLOCK = 128
EPS = 1e-6

MLA_HEADS = 8
MLA_NOPE = 64
MLA_ROPE = 32
MLA_V = 64
MLA_KV_RANK = 256
ROPE_THETA = 10000.0

DIL_PATTERNS = ((128, 1), (512, 4), (2048, 16))
N_DIL_GROUPS = 3
DIL_HEADS = 8
DIL_HEAD_DIM = 64

N_EXPERT_GROUPS = 4
EXPERTS_PER_GROUP = 8
N_EXPERTS = N_EXPERT_GROUPS * EXPERTS_PER_GROUP
TOP_K_EXPERT = 2
D_FF_EXPERT = 256

MLA_Q_COLS = MLA_HEADS * (MLA_NOPE + MLA_ROPE)
DIL_COLS = 3 * N_DIL_GROUPS * DIL_HEADS * DIL_HEAD_DIM
GATE_COLS = 2 * D_MODEL
IN_COLS = MLA_Q_COLS + MLA_KV_RANK + MLA_ROPE + DIL_COLS + GATE_COLS

kernel_name = 'hybrid_mla_dilated_gated_hmoe'


def rms_norm(x, g):
    xf = x.astype(jnp.float32)
    y = xf * lax.rsqrt(jnp.mean(xf * xf, axis=-1, keepdims=True) + EPS)
    return (y * g.astype(jnp.float32)).astype(x.dtype)


def alibi_slopes(n):
    return jnp.exp2(-8.0 * (jnp.arange(n, dtype=jnp.float32) + 1.0) / n)


def rope(t, positions):
    half = t.shape[-1] // 2
    freqs = ROPE_THETA ** (-jnp.arange(half, dtype=jnp.float32) / half)
    ang = positions.astype(jnp.float32)[:, :, None] * freqs
    cos = jnp.cos(ang)[:, :, None, :]
    sin = jnp.sin(ang)[:, :, None, :]
    t1 = t[..., :half].astype(jnp.float32)
    t2 = t[..., half:].astype(jnp.float32)
    return jnp.concatenate([t1 * cos - t2 * sin, t2 * cos + t1 * sin], axis=-1).astype(t.dtype)


def causal_block_attention(q, k, v, scale):
    B, S, H, dq = q.shape
    nb = S // BLOCK
    q_blocks = jnp.moveaxis(q.reshape(B, nb, BLOCK, H, dq), 1, 0)
    kpos = jnp.arange(S)

    def one_block(args):
        qb, bi = args
        s = jnp.einsum('bqhd,bkhd->bhqk', qb, k).astype(jnp.float32) * scale
        qpos = bi * BLOCK + jnp.arange(BLOCK)
        s = jnp.where(kpos[None, :] <= qpos[:, None], s, -jnp.inf)
        p = jax.nn.softmax(s, axis=-1)
        return jnp.einsum('bhqk,bkhd->bqhd', p.astype(v.dtype), v)

    o = lax.map(one_block, (q_blocks, jnp.arange(nb)))
    return jnp.moveaxis(o, 0, 1).reshape(B, S, H, v.shape[-1])


def banded_attention(q, k, v, n_back, stride, slopes, scale):
    assert n_back <= BLOCK
    N, L, H, dh = q.shape
    nb = -(-L // BLOCK)
    Lp = nb * BLOCK
    qb = jnp.pad(q, ((0, 0), (0, Lp - L), (0, 0), (0, 0))).reshape(N, nb, BLOCK, H, dh)

    def key_blocks(t):
        tp = jnp.pad(t, ((0, 0), (BLOCK, Lp - L), (0, 0), (0, 0)))
        prev = tp[:, :Lp].reshape(N, nb, BLOCK, H, dh)
        cur = tp[:, BLOCK:].reshape(N, nb, BLOCK, H, dh)
        return jnp.concatenate([prev, cur], axis=2)

    kb = key_blocks(k)
    vb = key_blocks(v)
    s = jnp.einsum('nbqhd,nbkhd->nbhqk', qb, kb).astype(jnp.float32) * scale
    i = jnp.arange(BLOCK)[:, None]
    j = jnp.arange(2 * BLOCK)[None, :]
    delta = i + BLOCK - j
    kpos = jnp.arange(nb)[:, None, None] * BLOCK - BLOCK + j[None]
    valid = (delta >= 0) & (delta <= n_back) & (kpos >= 0)
    bias = -slopes[:, None, None] * (delta * stride).astype(jnp.float32)
    s = jnp.where(valid[None, :, None], s + bias, -jnp.inf)
    m = jnp.max(s, axis=-1, keepdims=True)
    p = jnp.exp(s - m)
    den = jnp.sum(p, axis=-1, keepdims=True)
    o = jnp.einsum('nbhqk,nbkhd->nbqhd', (p / den).astype(v.dtype), vb)
    o = o.reshape(N, Lp, H, dh)[:, :L]
    lse = jnp.transpose((m + jnp.log(den))[..., 0], (0, 1, 3, 2)).reshape(N, Lp, H)[:, :L]
    return o, lse


def mla_branch(q_cols, ckv, k_rope, positions, norm_ckv, w_ukv, q_norm, k_norm):
    B, S, _ = q_cols.shape
    q = q_cols.reshape(B, S, MLA_HEADS, MLA_NOPE + MLA_ROPE)
    kv = (rms_norm(ckv, norm_ckv) @ w_ukv).reshape(B, S, MLA_HEADS, MLA_NOPE + MLA_V)
    k_nope, v = kv[..., :MLA_NOPE], kv[..., MLA_NOPE:]
    k = jnp.concatenate([k_nope, jnp.broadcast_to(k_rope[:, :, None, :], (B, S, MLA_HEADS, MLA_ROPE))], axis=-1)
    q = rms_norm(q, q_norm)
    k = rms_norm(k, k_norm)
    q = jnp.concatenate([q[..., :MLA_NOPE], rope(q[..., MLA_NOPE:], positions)], axis=-1)
    k = jnp.concatenate([k[..., :MLA_NOPE], rope(k[..., MLA_NOPE:], positions)], axis=-1)
    o = causal_block_attention(q, k, v, (MLA_NOPE + MLA_ROPE) ** -0.5)
    return o.reshape(B, S, MLA_HEADS * MLA_V)


def dilated_branch(qkv_cols, q_norm, k_norm):
    B, S, _ = qkv_cols.shape
    H, dh = DIL_HEADS, DIL_HEAD_DIM
    qkv = qkv_cols.reshape(B, S, 3, N_DIL_GROUPS, H, dh)
    q = rms_norm(qkv[:, :, 0], q_norm[:, None, :])
    k = rms_norm(qkv[:, :, 1], k_norm[:, None, :])
    v = qkv[:, :, 2]
    slopes = alibi_slopes(H)
    outs, lses = [], []
    for g, (window, dil) in enumerate(DIL_PATTERNS):
        L = S // dil
        def fold(t):
            return t[:, :, g].reshape(B, L, dil, H, dh).transpose(0, 2, 1, 3, 4).reshape(B * dil, L, H, dh)
        o, lse = banded_attention(fold(q), fold(k), fold(v), window // dil, dil, slopes, dh ** -0.5)
        outs.append(o.reshape(B, dil, L, H, dh).transpose(0, 2, 1, 3, 4).reshape(B, S, H, dh))
        lses.append(lse.reshape(B, dil, L, H).transpose(0, 2, 1, 3).reshape(B, S, H))
    w = jax.nn.softmax(jnp.stack(lses, axis=0), axis=0)
    o = jnp.sum(w[..., None] * jnp.stack(outs, axis=0).astype(jnp.float32), axis=0)
    return o.astype(qkv_cols.dtype).reshape(B, S, H * dh)


def hier_moe(h, w_rg, b_rg, w_re, b_re, w_gate, w_up, w_down):
    B, S, D = h.shape
    T = B * S
    hf = h.reshape(T, D)
    gp = jax.nn.softmax((hf @ w_rg + b_rg).astype(jnp.float32), axis=-1)
    g_p, g_i = lax.top_k(gp, 1)
    g_p, g_i = g_p[:, 0], g_i[:, 0]
    el = (hf @ w_re + b_re).astype(jnp.float32).reshape(T, N_EXPERT_GROUPS, EXPERTS_PER_GROUP)
    el_sel = el[jnp.arange(T), g_i]
    ep = jax.nn.softmax(el_sel, axis=-1)
    e_p, e_i = lax.top_k(ep, TOP_K_EXPERT)
    e_p = e_p / jnp.sum(e_p, axis=-1, keepdims=True)
    weights = g_p[:, None] * e_p
    expert_idx = g_i[:, None] * EXPERTS_PER_GROUP + e_i
    combine = jnp.sum(jax.nn.one_hot(expert_idx, N_EXPERTS, dtype=jnp.float32) * weights[..., None], axis=1)
    combine = combine.astype(hf.dtype)
    y = jnp.zeros_like(hf)
    for e in range(N_EXPERTS):
        he = jax.nn.silu(hf @ w_gate[e]) * (hf @ w_up[e])
        y = y + combine[:, e:e + 1] * (he @ w_down[e])
    return y.reshape(B, S, D)


def setup_inputs(seed: int = 0) -> dict:
    key = jax.random.key(seed)
    ks = jax.random.split(key, 24)

    def nrm(k, shape, fan_in):
        return jax.random.normal(k, shape, jnp.float32) * (fan_in ** -0.5)

    def gain(k, shape):
        return 1.0 + 0.02 * jax.random.normal(k, shape, jnp.float32)

    def bias(k, shape, scale):
        return scale * jax.random.normal(k, shape, jnp.float32)

    Ld = DEPTH
    return {
        'x': jax.random.normal(ks[0], (BATCH, SEQ, D_MODEL), jnp.float32),
        'positions': jnp.broadcast_to(jnp.arange(SEQ, dtype=jnp.int32), (BATCH, SEQ)),
        'norm_attn': gain(ks[1], (Ld, D_MODEL)),
        'w_in': nrm(ks[2], (Ld, D_MODEL, IN_COLS), D_MODEL),
        'b_gate': bias(ks[3], (Ld, 2, D_MODEL), 0.02),
        'norm_ckv': gain(ks[4], (Ld, MLA_KV_RANK)),
        'w_ukv': nrm(ks[5], (Ld, MLA_KV_RANK, MLA_HEADS * (MLA_NOPE + MLA_V)), MLA_KV_RANK),
        'q_norm_mla': gain(ks[6], (Ld, MLA_NOPE + MLA_ROPE)),
        'k_norm_mla': gain(ks[7], (Ld, MLA_NOPE + MLA_ROPE)),
        'q_norm_dil': gain(ks[8], (Ld, N_DIL_GROUPS, DIL_HEAD_DIM)),
        'k_norm_dil': gain(ks[9], (Ld, N_DIL_GROUPS, DIL_HEAD_DIM)),
        'w_o_mla': nrm(ks[10], (Ld, MLA_HEADS * MLA_V, D_MODEL), MLA_HEADS * MLA_V),
        'w_o_dil': nrm(ks[11], (Ld, DIL_HEADS * DIL_HEAD_DIM, D_MODEL), DIL_HEADS * DIL_HEAD_DIM),
        'w_out': nrm(ks[12], (Ld, D_MODEL, D_MODEL), D_MODEL),
        'norm_ffn': gain(ks[13], (Ld, D_MODEL)),
        'w_router_group': nrm(ks[14], (Ld, D_MODEL, N_EXPERT_GROUPS), D_MODEL),
        'b_router_group': bias(ks[15], (Ld, N_EXPERT_GROUPS), 0.01),
        'w_router_expert': nrm(ks[16], (Ld, D_MODEL, N_EXPERTS), D_MODEL),
        'b_router_expert': bias(ks[17], (Ld, N_EXPERTS), 0.01),
        'w_gate': nrm(ks[18], (Ld, N_EXPERTS, D_MODEL, D_FF_EXPERT), D_MODEL),
        'w_up': nrm(ks[19], (Ld, N_EXPERTS, D_MODEL, D_FF_EXPERT), D_MODEL),
        'w_down': nrm(ks[20], (Ld, N_EXPERTS, D_FF_EXPERT, D_MODEL), D_FF_EXPERT),
    }


def reference(x, positions, norm_attn, w_in, b_gate, norm_ckv, w_ukv, q_norm_mla, k_norm_mla,
              q_norm_dil, k_norm_dil, w_o_mla, w_o_dil, w_out, norm_ffn, w_router_group,
              b_router_group, w_router_expert, b_router_expert, w_gate, w_up, w_down):
    B, S, D = x.shape
    splits = [MLA_Q_COLS, MLA_Q_COLS + MLA_KV_RANK, MLA_Q_COLS + MLA_KV_RANK + MLA_ROPE,
              MLA_Q_COLS + MLA_KV_RANK + MLA_ROPE + DIL_COLS]
    for l in range(DEPTH):
        h = rms_norm(x, norm_attn[l])
        proj = h @ w_in[l]
        q_mla, ckv, k_rope, qkv_dil, gate_pre = jnp.split(proj, splits, axis=-1)
        o_mla = mla_branch(q_mla, ckv, k_rope, positions, norm_ckv[l], w_ukv[l],
                           q_norm_mla[l], k_norm_mla[l])
        o_dil = dilated_branch(qkv_dil, q_norm_dil[l], k_norm_dil[l])
        gates = jax.nn.sigmoid(gate_pre.reshape(B, S, 2, D) + b_gate[l])
        merged = gates[:, :, 0] * (o_mla @ w_o_mla[l]) + gates[:, :, 1] * (o_dil @ w_o_dil[l])
        x = x + merged @ w_out[l]
        h2 = rms_norm(x, norm_ffn[l])
        x = x + hier_moe(h2, w_router_group[l], b_router_group[l], w_router_expert[l],
                         b_router_expert[l], w_gate[l], w_up[l], w_down[l])
    return x
```

```python
import numpy as np
import concourse.bass as bass
import concourse.mybir as mybir

F32 = mybir.dt.float32
BF16 = mybir.dt.bfloat16
I32 = mybir.dt.int32
ALU = mybir.AluOpType
AF = mybir.ActivationFunctionType
AX = mybir.AxisListType


LAST_ALLOC = [None]
REG = []


class Sem:
    def __init__(self, h, name):
        self.h = h
        self.name = name
        self.count = 0


class Buf:
    def __init__(self, ap, name=""):
        self.ap = ap
        self.name = name
        self.w = None
        self.r = {}
        self.dsem = None
        if LAST_ALLOC[0] is not None:
            st, n = LAST_ALLOC[0]
            LAST_ALLOC[0] = None
            merged = {}
            keep = []
            for (s0, e0, ob) in REG:
                if s0 < st + n and st < e0:
                    evs = list(ob.r.items())
                    if ob.w is not None:
                        evs.append(ob.w)
                    for sem, val in evs:
                        if merged.get(sem, 0) < val:
                            merged[sem] = val
                else:
                    keep.append((s0, e0, ob))
            keep.append((st, st + n, self))
            REG[:] = keep
            self.r = merged

    def __getitem__(self, idx):
        return View(self, self.ap[idx])

    def v(self, ap=None):
        return View(self, self.ap if ap is None else ap)


class View:
    def __init__(self, buf, ap):
        self.buf = buf
        self.ap = ap

    def __getitem__(self, idx):
        return View(self.buf, self.ap[idx])

    def re(self, s, **kw):
        return View(self.buf, self.ap.rearrange(s, **kw))

    def bc(self, shape):
        return View(self.buf, self.ap.broadcast_to(shape))


class Eng:
    def __init__(self, name, sem):
        self.name = name
        self.sem = sem
        self.ops = []
        self.known = {}
        self.pending = False


class FW:
    def __init__(self, nc, stack):
        self.nc = nc
        self.stack = stack
        self.nsem = 0
        self.E = {}
        for n in ("pe", "act", "dve", "pool", "sp"):
            self.E[n] = Eng(n, self.new_sem("s_" + n))
        self.out_events = []

    def new_sem(self, name):
        h = self.stack.enter_context(self.nc.semaphore(name))
        self.nsem += 1
        return Sem(h, name)

    def sbuf(self, name, shape, dtype):
        t = self.stack.enter_context(self.nc.sbuf_tensor(name, list(shape), dtype))
        return t

    def psum(self, name, shape, dtype):
        t = self.stack.enter_context(self.nc.psum_tensor(name, list(shape), dtype))
        return t

    def _waits(self, E, reads, writes):
        need = {}

        def add(ev, same_engine_ok):
            if ev is None:
                return
            sem, val = ev
            if sem is E.sem and same_engine_ok:
                return
            if need.get(sem, 0) < val:
                need[sem] = val

        for v in reads:
            add(v.buf.w, False)
        for v in writes:
            add(v.buf.w, True)
            for sem, val in v.buf.r.items():
                add((sem, val), True)
        out = []
        for sem, val in need.items():
            if E.known.get(sem, 0) >= val:
                continue
            E.known[sem] = val
            out.append((sem, val))
        return out

    def _record(self, ev, reads, writes):
        sem, val = ev
        for v in reads:
            b = v.buf
            if b.r.get(sem, 0) < val:
                b.r[sem] = val
        for v in writes:
            b = v.buf
            b.w = ev
            b.r = {}

    def op(self, eng, make, reads=(), writes=(), signal=True):
        E = self.E[eng]
        waits = self._waits(E, reads, writes)
        val = E.sem.count + 1
        if signal:
            E.sem.count = val
            E.pending = False
        else:
            E.pending = True
        semh = E.sem.h

        def run(e, waits=waits, make=make, signal=signal, semh=semh):
            for s, v in waits:
                e.wait_ge(s.h, v)
            ins = make(e)
            if signal:
                ins.then_inc(semh, 1)

        E.ops.append(run)
        self._record((E.sem, val), reads, writes)

    def dma(self, q, out, in_, out_is_dram=False, in_is_dram=False, sem=None, **kw):
        E = self.E[q]
        reads = [] if in_is_dram else [in_]
        writes = [] if out_is_dram else [out]
        waits = self._waits(E, reads, writes)
        if sem is None:
            b = (writes[0] if writes else reads[0]).buf
            if b.dsem is None:
                b.dsem = self.new_sem("d_" + b.name)
            sem = b.dsem
        sem.count += 16
        val = sem.count
        oap = out if out_is_dram else out.ap
        iap = in_ if in_is_dram else in_.ap

        def run(e, waits=waits, oap=oap, iap=iap, semh=sem.h, kw=kw):
            for s, v in waits:
                e.wait_ge(s.h, v)
            e.dma_start(out=oap, in_=iap, **kw).then_inc(semh, 16)

        E.ops.append(run)
        self._record((sem, val), reads, writes)
        if out_is_dram:
            self.out_events.append((sem, val))
        return (sem, val)

    def finish(self):
        E = self.E["sp"]
        final = {}
        for sem, val in self.out_events:
            final[sem] = max(final.get(sem, 0), val)
        fl = list(final.items())

        def run(e, fl=fl):
            for s, v in fl:
                e.wait_ge(s.h, v)

        E.ops.append(run)
        nc = self.nc
        with nc.Block() as block:
            @block.tensor
            def _(e):
                for f in self.E["pe"].ops:
                    f(e)

            @block.scalar
            def _(e):
                for f in self.E["act"].ops:
                    f(e)

            @block.vector
            def _(e):
                for f in self.E["dve"].ops:
                    f(e)

            @block.gpsimd
            def _(e):
                for f in self.E["pool"].ops:
                    f(e)

            @block.sync
            def _(e):
                for f in self.E["sp"].ops:
                    f(e)

    def matmul(self, out, lhsT, rhs, start=True, stop=True, signal=True, extra_reads=(), **kw):
        self.op("pe", lambda e: e.matmul(out.ap, lhsT.ap, rhs.ap, start=start, stop=stop, **kw),
                reads=[lhsT, rhs, *extra_reads], writes=[out], signal=signal)

    def transpose(self, out, in_, ident, signal=True):
        self.op("pe", lambda e: e.transpose(out.ap, in_.ap, ident.ap),
                reads=[in_, ident], writes=[out], signal=signal)

    def act(self, out, in_, func, bias=None, scale=1.0, accum_out=None, eng="act"):
        reads = [in_]
        kw = {}
        if bias is not None:
            if isinstance(bias, View):
                reads.append(bias)
                kw["bias"] = bias.ap
            else:
                kw["bias"] = bias
        if isinstance(scale, View):
            reads.append(scale)
            sc = scale.ap
        else:
            sc = scale
        writes = [out]
        if accum_out is not None:
            writes.append(accum_out)
            kw["accum_out"] = accum_out.ap
        self.op("act", lambda e: e.activation(out.ap, in_.ap, func, scale=sc, **kw),
                reads=reads, writes=writes)

    def tscalar(self, eng, out, in0, s1, s2, op0, op1=None, accum_out=None):
        reads = [in0]
        a1 = s1
        a2 = s2
        if isinstance(s1, View):
            reads.append(s1)
            a1 = s1.ap
        if isinstance(s2, View):
            reads.append(s2)
            a2 = s2.ap
        kw = {}
        writes = [out]
        if op1 is not None:
            kw["op1"] = op1
        if accum_out is not None:
            kw["accum_out"] = accum_out.ap
            writes.append(accum_out)
        self.op(eng, lambda e: e.tensor_scalar(out.ap, in0.ap, a1, a2, op0, **kw),
                reads=reads, writes=writes)

    def tt(self, eng, out, in0, in1, op):
        self.op(eng, lambda e: e.tensor_tensor(out.ap, in0.ap, in1.ap, op),
                reads=[in0, in1], writes=[out])

    def stt(self, eng, out, in0, scalar, in1, op0, op1):
        reads = [in0, in1]
        sc = scalar
        if isinstance(scalar, View):
            reads.append(scalar)
            sc = scalar.ap
        self.op(eng, lambda e: e.scalar_tensor_tensor(out.ap, in0.ap, sc, in1.ap, op0, op1),
                reads=reads, writes=[out])

    def copy(self, eng, out, in_):
        if eng == "act":
            self.op("act", lambda e: e.copy(out.ap, in_.ap), reads=[in_], writes=[out])
        else:
            self.op(eng, lambda e: e.tensor_copy(out.ap, in_.ap), reads=[in_], writes=[out])

    def memset(self, eng, out, val):
        self.op(eng, lambda e: e.memset(out.ap, val), reads=[], writes=[out])

    def reduce(self, eng, out, in_, op, axis=None):
        axis = AX.X if axis is None else axis
        self.op(eng, lambda e: e.tensor_reduce(out.ap, in_.ap, axis, op), reads=[in_], writes=[out])

    def recip(self, out, in_):
        self.op("dve", lambda e: e.reciprocal(out.ap, in_.ap), reads=[in_], writes=[out])


import math
from contextlib import ExitStack
from concourse.bass_utils import run_bass_kernel_spmd

S = 2048
DM = 1024
NT = 16
EPS = 1e-6
DILB = 1056
GATEB = 1056 + 4608
DILS = (1, 4, 16)
NBLK = (16, 4, 1)
U8 = mybir.dt.uint8

C_GA, C_GF, C_GC, C_BG, C_GQM, C_GKM, C_GQD, C_GKD, C_FRQ, C_BR = 0, 8, 16, 18, 34, 35, 36, 39, 42, 43
NCA = 79
M_ID, M_ONES, M_BLK, M_O96, M_PT, M_ISH, M_CAUS = 0, 128, 256, 384, 480, 576, 672
NCM = 800


def _sz(dt):
    return {F32: 4, BF16: 2, I32: 4}[dt]


def shaped(ap, shape):
    if len(shape) == 1:
        return ap
    names = "abcd"[:len(shape)]
    pat = "p (" + " ".join(names) + ") -> p " + " ".join(names)
    kw = {n: s for n, s in zip(names[:-1], shape[:-1])}
    return ap.rearrange(pat, **kw)


class Arena:
    def __init__(self, t, base, size):
        self.t, self.base, self.off, self.end = t, base, base, base + size

    def reset(self, to=None):
        self.off = self.base if to is None else to

    def alloc(self, shape, dt):
        n = _sz(dt)
        for s in shape:
            n *= s
        n = (n + 63) // 64 * 64
        assert self.off + n <= self.end, ("arena overflow", self.off, n, self.end)
        ap = self.t[:, self.off:self.off + n]
        self.off += n
        nb = _sz(dt)
        tot = 1
        for s in shape:
            tot *= s
        ap = self.t[:, self.off - n:self.off - n + tot * nb].bitcast(dt)
        LAST_ALLOC[0] = (self.off - n, n)
        return shaped(ap, shape)


ACTIVE = {"HOLD"}
CUR = [None]
TOKC = [0]


class Rot:
    def __init__(self, bufs, name="rot"):
        self.bufs = bufs
        self.i = 0
        self.busy = [None] * len(bufs)
        self.name = name

    def next(self):
        n = len(self.bufs)
        for k in range(n):
            j = (self.i + k) % n
            o = self.busy[j]
            if o is None or o not in ACTIVE:
                self.busy[j] = CUR[0]
                self.i = j + 1
                return self.bufs[j]
        raise RuntimeError("rotation too shallow: %s (size %d)" % (self.name, n))

    def release(self, buf):
        for j, b in enumerate(self.bufs):
            if b is buf:
                self.busy[j] = None

    def hold(self, buf):
        for j, b in enumerate(self.bufs):
            if b is buf:
                self.busy[j] = "HOLD"


def fw_barrier(fw):
    sems = fw.sems
    for E in fw.E.values():
        assert not E.pending, E.name
        waits = []
        for s in sems:
            if s is E.sem:
                continue
            if s.count > E.known.get(s, 0):
                E.known[s] = s.count
                waits.append((s, s.count))

        def run(e, waits=waits):
            for s, v in waits:
                e.wait_ge(s.h, v)
        E.ops.append(run)


import os
PIPE_MAX = int(os.environ.get('K_PIPE', '8'))
K_LN = os.environ.get('K_LN', '1') == '1'
K_DM = int(os.environ.get('K_DM', '1'))
K_DD = int(os.environ.get('K_DD', '2'))
K_DP = int(os.environ.get('K_DP', '2'))
K_D3 = int(os.environ.get('K_D3', '2'))


def build_nc(NSEQ=2, dbg=None, stop=None):
    nc = bass.Bass("TRN2", target_bir_lowering=False)
    dr = lambda name, shape, dt=F32: nc.dram_tensor(name, list(shape), dt, kind="ExternalInput").ap()
    x_d = dr("x", [NSEQ, S, DM])
    pos_d = dr("pos", [NSEQ, S], I32)
    win_d = dr("w_in", [DM, 7712])
    wukv_d = dr("w_ukv", [256, 1024])
    woa_d = dr("w_o_mla", [512, 1024])
    wob_d = dr("w_o_dil", [512, 1024])
    wout_d = dr("w_out", [DM, DM])
    wg_d = dr("w_gate", [32, DM, 256])
    wu_d = dr("w_up", [32, DM, 256])
    wd_d = dr("w_down", [32, 256, DM])
    wr_d = dr("wr", [DM, 36])
    ca_d = dr("cst_a", [128, NCA])
    cm_d = dr("cst_m", [128, NCM])
    e_d = dr("cst_e", [65, 64])
    mt_d = dr("mtab", [3, 128, 8, 256])
    y_d = nc.dram_tensor("y", [NSEQ, S, DM], F32, kind="ExternalOutput").ap()
    dbg_out = {}

    with ExitStack() as st:
        fw = FW(nc, st)
        fw.sems = [e.sem for e in fw.E.values()]
        csem = fw.new_sem("csem")
        ssem = fw.new_sem("ssem")
        def newc():
            sm = fw.new_sem("cs%d" % fw.nsem)
            fw.sems.append(sm)
            return sm
        lsem = {"pool": [fw.new_sem("lp%d" % i) for i in range(14)], "sp": [fw.new_sem("ls%d" % i) for i in range(6)]}
        fw.sems += [csem, ssem] + lsem["pool"] + lsem["sp"]
        lidx = {"pool": 0, "sp": 0}

        def load(q, out, in_, **kw):
            s = lsem[q][lidx[q] % len(lsem[q])]
            lidx[q] += 1
            fw.dma(q, out, in_, in_is_dram=True, sem=s, **kw)

        def pipeline(gens, depth, warm=None):
            depth = min(depth, PIPE_MAX)
            active = []
            tok = {}
            it = iter(gens)
            done = False
            while True:
                for g_ in list(active):
                    CUR[0] = tok[id(g_)]
                    try:
                        next(g_)
                    except StopIteration:
                        active.remove(g_)
                        ACTIVE.discard(tok.pop(id(g_)))
                if not done and len(active) < depth:
                    try:
                        g_ = next(it)
                        TOKC[0] += 1
                        tok[id(g_)] = TOKC[0]
                        CUR[0] = TOKC[0]
                        ACTIVE.add(TOKC[0])
                        try:
                            next(g_)
                            active.append(g_)
                        except StopIteration:
                            ACTIVE.discard(tok.pop(id(g_)))
                    except StopIteration:
                        done = True
                CUR[0] = None
                if warm is not None:
                    for _ in range(warm[1]):
                        fw.matmul(warm[0][:, 0:512], ident, cM[:, 0:512])
                if done and not active:
                    break

        def pers(name, shape, dt):
            LAST_ALLOC[0] = None
            return Buf(fw.sbuf(name, shape, dt)[:], name)
        cA = pers("cA", [128, NCA], F32)
        cM = pers("cM", [128, NCM], BF16)
        Ef = pers("Ef", [65, 64], F32)
        Wkr = pers("Wkr", [128, 8, 96], BF16)
        wkk = pers("wkk", [128, 2, 8, 96], BF16)
        wkv = pers("wkv", [128, 2, 8, 64], BF16)
        wrb = pers("wrb", [128, 8, 36], BF16)
        epsc = pers("epsc", [128, 4], F32)
        gsc = pers("gsc", [128, 8], F32)
        comb = pers("comb", [128, NT, 32], F32)
        LG = pers("LG", [128, NT, 36], F32)
        RS = pers("RS", [128, 16, 64], F32)
        arena_t = fw.sbuf("arena", [128, 190464], U8)
        A_OFF, B_OFF, C_OFF, D_OFF = 0, 32768, 65536, 98304
        arD = Arena(arena_t, D_OFF, 92160)
        arB = Arena(arena_t, B_OFF, 32768)
        arC = Arena(arena_t, C_OFF, 32768)
        arAB = Arena(arena_t, A_OFF, 65536)
        pst = [fw.psum("ps%d" % i, [128, 1024], F32) for i in range(4)]
        LAST_ALLOC[0] = None
        PB = [Buf(pst[i // 2][:, (i % 2) * 512:(i % 2) * 512 + 512], "pb%d" % i) for i in range(8)]
        PW = [Buf(pst[i][:], "pw%d" % i) for i in range(4)]

        fw.dma("sp", cA.v(), ca_d, in_is_dram=True, sem=newc())
        fw.dma("sp", Ef.v(), e_d, in_is_dram=True, sem=newc())
        fw.dma("pool", cM.v(), cm_d, in_is_dram=True, sem=newc())
        fw.memset("dve", Wkr.v(), 0.0)
        fw.memset("dve", wkk.v(), 0.0)
        fw.dma("pool", Wkr[:, :, 64:96], win_d[:, 1024:1056].rearrange("(c p) n -> p c n", p=128), in_is_dram=True, sem=newc())
        ukv = wukv_d.rearrange("(c p) (h d) -> p c h d", p=128, d=128)
        for c in range(2):
            fw.dma("pool", wkk[:, c, :, 0:64], ukv[:, c, :, 0:64], in_is_dram=True, sem=newc())
            fw.dma("pool", wkv[:, c, :, :], ukv[:, c, :, 64:128], in_is_dram=True, sem=newc())
        fw.dma("pool", wrb.v(), wr_d.rearrange("(c p) n -> p c n", p=128), in_is_dram=True, sem=newc())
        fw.memset("dve", epsc[:, 0:1], EPS)
        fw.memset("dve", epsc[:, 1:2], 96 * EPS)
        fw.memset("dve", epsc[:, 2:3], 64 * EPS)
        fw.memset("dve", epsc[:, 3:4], 0.0)
        fw_barrier(fw)
        fw.tscalar("dve", gsc[:, 0:2], cA[:, C_GQM:C_GQM + 2], math.sqrt(96.0), None, ALU.mult)
        fw.tscalar("dve", gsc[:, 2:8], cA[:, C_GQD:C_GQD + 6], 8.0, None, ALU.mult)
        fw_barrier(fw)

        ident = cM[:, M_ID:M_ID + 128]
        ones128 = cM[:, M_ONES:M_ONES + 128]
        onesblk = cM[:, M_BLK:M_BLK + 128]
        ones96 = cM[0:96, M_O96:M_O96 + 96]
        PTm = cM[0:96, M_PT:M_PT + 96]
        ISH = cM[0:96, M_ISH:M_ISH + 96]
        caus = cM[:, M_CAUS:M_CAUS + 128]

        def slab_load(dst, c0, ncols):
            load("pool", dst, win_d[:, c0:c0 + ncols].rearrange("(c p) n -> p c n", p=128))


        def norm_gen(raws, nrows, ones_l, eps_col, inv_n, gcols, outs, ssq_rot, sq_rot, sf_rot, xf=None):
            n = len(raws)
            qs = []
            for i, r in enumerate(raws):
                q = sq_rot.next()
                fw.act(q[0:nrows, :], r, AF.Square)
                qs.append(q)
            yield
            ssq = ssq_rot.next()
            for i, q in enumerate(qs):
                fw.matmul(ssq[0:nrows, :], ones_l, q[0:nrows, :], start=(i == 0), stop=(i == n - 1), signal=(i == n - 1))
            yield
            sv = sf_rot.next()
            if K_LN:
                fw.act(sv[0:nrows, :], ssq[0:nrows, :], AF.Ln, scale=inv_n, bias=epsc[0:nrows, eps_col:eps_col + 1])
                ssq_rot.release(ssq)
                fw.act(sv[0:nrows, :], sv[0:nrows, :], AF.Exp, scale=-0.5)
            else:
                fw.act(sv[0:nrows, :], ssq[0:nrows, :], AF.Sqrt, scale=inv_n, bias=epsc[0:nrows, eps_col:eps_col + 1])
                ssq_rot.release(ssq)
                yield
                fw.recip(sv[0:nrows, :], sv[0:nrows, :])
            yield
            for r, g, o in zip(raws, gcols, outs):
                if xf is None:
                    fw.stt("dve", o, r, g, sv[0:nrows, :], ALU.mult, ALU.mult)
                else:
                    fw.stt("dve", o, xf(r), g, xf(sv[0:nrows, :]), ALU.mult, ALU.mult)

        for s in range(NSEQ):
            LAST_ALLOC[0] = (A_OFF, 32768)
            hT = Buf(shaped(arena_t[:, A_OFF:A_OFF + 32768].bitcast(BF16), [8, S]), "hT")
            arD.reset()
            xts = Rot([Buf(arD.alloc([DM], F32), "xt%d" % i) for i in range(4)], "r1")
            junk = Buf(arD.alloc([DM], BF16), "junk")
            xbs = Rot([Buf(arD.alloc([DM], BF16), "xb%d" % i) for i in range(4)], "r2")
            sts = Rot([Buf(arD.alloc([4], F32), "st%d" % i) for i in range(4)], "r3")
            t_ps = Rot(PB[0:4])
            gA_bc = View(cA, cA.ap[:, C_GA:C_GA + 8].unsqueeze(2).broadcast_to([128, 8, 128]))

            def p0_item(t):
                xt = xts.next()
                load("sp", xt.v(), x_d[s, t * 128:(t + 1) * 128, :])
                stt_ = sts.next()
                fw.act(junk.v(), xt.v(), AF.Square, accum_out=stt_[:, 0:1])
                yield
                fw.act(stt_[:, 1:2], stt_[:, 0:1], AF.Sqrt, scale=1.0 / DM, bias=epsc[:, 0:1])
                yield
                fw.recip(stt_[:, 2:3], stt_[:, 1:2])
                yield
                xb = xbs.next()
                fw.tscalar("dve", xb.v(), xt.v(), stt_[:, 2:3], None, ALU.mult)
                yield
                pb = t_ps.next()
                pbv = View(pb, shaped(pb.ap.bitcast(BF16), [8, 128]))
                for k in range(8):
                    fw.transpose(pbv[:, k, :], xb[:, k * 128:(k + 1) * 128], ident, signal=(k == 7))
                yield
                fw.tt("dve", hT[:, :, t * 128:(t + 1) * 128], pbv, gA_bc, ALU.mult)
            pipeline((p0_item(t) for t in range(NT)), 4)
            if stop == 0:
                fw.finish()
                return nc, dbg_out

            LAST_ALLOC[0] = (B_OFF, 32768)
            omla = Buf(shaped(arena_t[:, B_OFF:B_OFF + 32768].bitcast(BF16), [8, S]), "omla")
            arD.reset()
            arC.reset()
            qT = [Buf(arD.alloc([S], BF16), "qT") for _ in range(8)]
            kT = [Buf(arD.alloc([S], BF16), "kT") for _ in range(8)]
            vaug = Buf(arC.alloc([NT, 8, 65], BF16), "vaug")
            wq = Buf(arC.alloc([8, 768], BF16), "wq")
            markD = arD.off
            wc = Buf(arD.alloc([8, 256], BF16), "wc")
            slab_load(wq.v(), 0, 768)
            slab_load(wc.v(), 768, 256)
            fw.memset("pool", vaug[:, :, :, 64:65], 1.0)
            ckvn_r = Rot([Buf(arD.alloc([2, 512], BF16), "ckvn%d" % i) for i in range(2)], "r4")
            krs_r = Rot([Buf(arD.alloc([512], BF16), "krs%d" % i) for i in range(2)], "r5")
            arB.reset()
            sqr = Rot([Buf(arB.alloc([512], BF16), "sq%d" % i) for i in range(4)], "r6")
            sfr = Rot([Buf(arB.alloc([512], F32), "sf%d" % i) for i in range(3)], "r7")
            ropeC_r = Rot([Buf(arB.alloc([512], F32), "ropeC%d" % i) for i in range(2)], "r8")
            ropeS_r = Rot([Buf(arB.alloc([512], F32), "ropeS%d" % i) for i in range(2)], "r9")
            posi = Buf(arB.alloc([512], I32), "posi")
            angb = Buf(arB.alloc([512], F32), "ang")
            tqb = Buf(arB.alloc([512], F32), "tq")
            kib = posi
            kfb = Buf(arB.alloc([512], F32), "kf")
            rtmp = Rot([Buf(arD.alloc([512], F32), "rt%d" % i) for i in range(2)], "r10")
            rtm2 = Rot([Buf(arD.alloc([512], F32), "ru%d" % i) for i in range(2)], "r11")
            ssq_ps = Rot(PB[4:6], "m_ssq")
            rot_ps = Rot(PB[6:8], "m_rot")
            raw_ps = Rot(PB[0:4], "m_raw")
            R = slice(64, 96)
            TWO_PI = 2.0 * math.pi

            def sincos(out, phase):
                fw.tscalar("dve", tqb[R, :], angb[R, :], phase, 1.0 / TWO_PI, ALU.add, ALU.mult)
                fw.copy("dve", kib[R, :], tqb[R, :])
                fw.copy("dve", kfb[R, :], kib[R, :])
                fw.tscalar("dve", tqb[R, :], angb[R, :], phase, None, ALU.add)
                fw.stt("dve", tqb[R, :], kfb[R, :], -TWO_PI, tqb[R, :], ALU.mult, ALU.add)
                fw.tscalar("dve", kfb[R, :], tqb[R, :], math.pi, TWO_PI, ALU.is_gt, ALU.mult)
                fw.tt("dve", tqb[R, :], tqb[R, :], kfb[R, :], ALU.subtract)
                fw.tscalar("dve", kfb[R, :], tqb[R, :], -math.pi, TWO_PI, ALU.is_lt, ALU.mult)
                fw.tt("dve", tqb[R, :], tqb[R, :], kfb[R, :], ALU.add)
                fw.act(out[R, :], tqb[R, :], AF.Sin)

            tabs = {}

            def rope_item(tg):
                cols = slice(tg * 512, (tg + 1) * 512)
                load("sp", posi[0:96, :], pos_d[s:s + 1, cols].partition_broadcast(96))
                fw.copy("dve", angb[R, :], posi[R, :])
                fw.tscalar("dve", angb[R, :], angb[R, :], cA[R, C_FRQ:C_FRQ + 1], None, ALU.mult)
                yield
                S_ = ropeS_r.next()
                C_ = ropeC_r.next()
                sincos(S_, 0.0)
                yield
                sincos(C_, math.pi / 2)
                tabs[tg] = (C_, S_)

            def normrope_item(tg, mk_raw, gcol, out):
                rp = mk_raw()
                yield
                yield from norm_gen([rp[0:96, :]], 96, ones96, 1, 1.0, [gcol], [out], ssq_ps, sqr, sfr)
                raw_ps.release(rp)
                yield
                rp2 = rot_ps.next()
                fw.matmul(rp2[0:96, :], PTm, out)
                yield
                C_, S_ = tabs[tg]
                t1 = rtmp.next()
                t2 = rtm2.next()
                fw.tt("dve", t1[R, :], rp2[R, :], S_[R, :], ALU.mult)
                rot_ps.release(rp2)
                fw.tt("pool", t2[R, :], out[R, :], C_[R, :], ALU.mult)
                yield
                fw.tt("pool", out[R, :], t2[R, :], t1[R, :], ALU.add)

            cur = {}

            def ckv_item(tg):
                cols = slice(tg * 512, (tg + 1) * 512)
                raws = []
                for c in range(2):
                    rp = raw_ps.next()
                    for k in range(8):
                        fw.matmul(rp.v(), wc[:, k, c * 128:(c + 1) * 128], hT[:, k, cols], start=(k == 0), stop=(k == 7), signal=(k == 7))
                    raws.append(rp.v())
                ck = ckvn_r.next()
                ckvn_r.release(ck)
                cur[("ckvn", tg)] = ck
                yield
                yield from norm_gen(raws, 128, ones128, 0, 1.0 / 256, [cA[:, C_GC:C_GC + 1], cA[:, C_GC + 1:C_GC + 2]],
                                    [ck[:, 0, :], ck[:, 1, :]], ssq_ps, sqr, sfr)
                for r_ in raws:
                    raw_ps.release(r_.buf)

            def kr_item(tg):
                cols = slice(tg * 512, (tg + 1) * 512)
                rp = raw_ps.next()
                for k in range(8):
                    fw.matmul(rp[0:96, :], Wkr[:, k, :], hT[:, k, cols], start=(k == 0), stop=(k == 7), signal=(k == 7))
                kr = krs_r.next()
                krs_r.release(kr)
                cur[("krs", tg)] = kr
                yield
                fw.copy("act", kr[0:96, :], rp[0:96, :])

            def mk_k(tg, h):
                def f():
                    ck = cur[("ckvn", tg)]
                    kr = cur[("krs", tg)]
                    rp = raw_ps.next()
                    for c in range(2):
                        fw.matmul(rp[0:96, :], wkk[:, c, h, :], ck[:, c, :], start=(c == 0), stop=False, signal=False)
                    fw.matmul(rp[0:96, :], ISH, kr[0:96, :], start=False, stop=True)
                    return rp
                return f

            def mk_q(tg, h):
                def f():
                    cols = slice(tg * 512, (tg + 1) * 512)
                    rp = raw_ps.next()
                    for k in range(8):
                        fw.matmul(rp[0:96, :], wq[:, k, h * 96:(h + 1) * 96], hT[:, k, cols], start=(k == 0), stop=(k == 7), signal=(k == 7))
                    return rp
                return f

            def v_item(tg, t4):
                ck = cur[("ckvn", tg)]
                rp = raw_ps.next()
                for c in range(2):
                    fw.matmul(rp.v(), ck[:, c, t4 * 128:(t4 + 1) * 128], wkv[:, c, :, :].re("p h d -> p (h d)"),
                              start=(c == 0), stop=(c == 1), signal=(c == 1))
                yield
                fw.copy("act", vaug[:, tg * 4 + t4, :, 0:64], rp.v().re("p (h d) -> p h d", h=8))

            def mla_prep_items():
                yield rope_item(0)
                for tg in range(4):
                    cols = slice(tg * 512, (tg + 1) * 512)
                    yield ckv_item(tg)
                    yield kr_item(tg)
                    for h in range(4):
                        yield normrope_item(tg, mk_q(tg, h), gsc[0:96, 0:1], qT[h][0:96, cols])
                    if tg + 1 < 4:
                        yield rope_item(tg + 1)
                    for h in range(8):
                        yield normrope_item(tg, mk_k(tg, h), gsc[0:96, 1:2], kT[h][0:96, cols])
                        if h + 4 < 8:
                            yield normrope_item(tg, mk_q(tg, h + 4), gsc[0:96, 0:1], qT[h + 4][0:96, cols])
                    for t4 in range(4):
                        yield v_item(tg, t4)
            pipeline(mla_prep_items(), 3)
            if stop == 1:
                fw.finish()
                return nc, dbg_out

            arD.reset(markD)
            er = Rot([Buf(arD.alloc([512], BF16), "e%d" % i) for i in range(7)], "r12")
            utr = Rot([Buf(arD.alloc([512], F32), "ut%d" % i) for i in range(2)], "r13")
            rdr = Rot([Buf(arD.alloc([512], F32), "rd%d" % i) for i in range(2)], "r14")
            s_ps = Rot(PB[0:4], "a_s")
            zbank = PB[4]
            u_ps = Rot(PB[5:7], "a_u")
            b_ps = Rot(PB[7:8], "a_b")
            scale_m = 96.0 ** -0.5
            ubank = {}

            def mla_item(h, j, kt):
                nq0 = max(0, kt - 4 * j) * 128
                N = 512 - nq0
                sp_ = s_ps.next()
                fw.matmul(sp_[:, 0:N], kT[h][0:96, kt * 128:(kt + 1) * 128], qT[h][0:96, j * 512 + nq0:(j + 1) * 512])
                yield
                e = er.next()
                fw.act(e[:, 0:N], sp_[:, 0:N], AF.Exp, scale=scale_m)
                s_ps.release(sp_)
                yield
                if kt >= 4 * j:
                    fw.tt("pool", e[:, 0:128], e[:, 0:128], caus, ALU.mult)
                    yield
                for _ in range(K_DM):
                    fw.matmul(zbank[:, 0:512], ident, cM[:, 0:512], signal=False)
                if kt == 0:
                    ubank[(h, j)] = u_ps.next()
                up = ubank[(h, j)]
                last = (kt == 4 * j + 3)
                fw.matmul(up[0:65, nq0:512], vaug[:, kt, h, :], e[:, 0:N], start=(kt == 0), stop=last, signal=last)
                er.release(e)
                if last:
                    yield
                    ut = utr.next()
                    fw.copy("act", ut[0:65, :], up[0:65, :])
                    yield
                    bp = b_ps.next()
                    fw.matmul(bp[0:64, :], Ef.v(), ut[0:65, :])
                    yield
                    rd = rdr.next()
                    fw.act(rd[0:64, :], bp[0:64, :], AF.Ln)
                    yield
                    fw.act(rd[0:64, :], rd[0:64, :], AF.Exp, scale=-1.0)
                    yield
                    fw.tt("dve", omla[0:64, h, j * 512:(j + 1) * 512], ut[0:64, :], rd[0:64, :], ALU.mult)
            pipeline((mla_item(h, j, kt) for h in range(8) for j in range(4) for kt in range(4 * j + 4)), 4)
            if stop == 2:
                fw.finish()
                return nc, dbg_out

            LAST_ALLOC[0] = (C_OFF, 32768)
            odil = Buf(shaped(arena_t[:, C_OFF:C_OFF + 32768].bitcast(BF16), [8, S]), "odil")
            for hh in range(2):
                arD.reset()
                Ut = [Buf(arD.alloc([S], F32), "Ut") for _ in range(4)]
                wqg = Buf(arD.alloc([8, 256], BF16), "wqg")
                wkg = Buf(arD.alloc([8, 256], BF16), "wkg")
                wvg = Buf(arD.alloc([8, 256], BF16), "wvg")
                mtbs = [Buf(arD.alloc([4, 256], BF16), "mtb%d" % i) for i in range(2)]
                markU = arD.off

                def dil_loads(g_):
                    for which, dst in enumerate((wqg, wkg, wvg)):
                        slab_load(dst.v(), DILB + which * 1536 + g_ * 512 + hh * 256, 256)
                    load("pool", mtbs[g_ % 2].v(), mt_d[g_, :, hh * 4:hh * 4 + 4, :])
                dil_loads(0)
                qg = Buf(arD.alloc([2, S], BF16), "qg")
                kg = Buf(arD.alloc([2, S], BF16), "kg")
                vg = Buf(arD.alloc([NT, 4, 65], BF16), "vg")
                fw.memset("pool", vg[:, :, :, 64:65], 1.0)
                sqr = Rot([Buf(arD.alloc([512], BF16), "sq%d" % i) for i in range(4)], "r15")
                sfr = Rot([Buf(arD.alloc([512], F32), "sf%d" % i) for i in range(3)], "r16")
                er = Rot([Buf(arD.alloc([512], BF16), "e%d" % i) for i in range(7)], "r17")
                for g in range(3):
                    dil = DILS[g]
                    nb = NBLK[g]
                    mtb = mtbs[g % 2]
                    ssq_ps = Rot(PB[4:6], "d_ssq")
                    raw_ps = Rot(PB[0:4] + PB[6:7], "d_raw")

                    def hview(k, j0, nt):
                        if g == 0:
                            return hT[:, k, j0 * 128:(j0 + nt) * 128]
                        if g == 1:
                            r, b = j0 // 4, j0 % 4
                            st0 = r + 512 * b
                            return hT[:, k, st0:st0 + 512 * nt - 3:4] if nt < 4 else hT[:, k, r:S:4]
                        if nt == 1:
                            return hT[:, k, j0:S:16]
                        return View(hT, hT.ap[:, k, :].rearrange("p (i s) -> p s i", s=16)[:, j0:j0 + nt, :])

                    def qk_item(tg, wsl, gi, dstb, c):
                        cols = slice(tg * 512, (tg + 1) * 512)
                        rp = raw_ps.next()
                        for k in range(8):
                            fw.matmul(rp.v(), wsl[:, k, c * 128:(c + 1) * 128], hT[:, k, cols],
                                      start=(k == 0), stop=(k == 7), signal=(k == 7))
                        yield
                        if g == 0:
                            outv, xf = dstb[:, c, cols], None
                        else:
                            rr = dil
                            un = 512 // rr
                            outv = dstb[:, c, :].re("p (r u) -> p r u", r=rr)[:, :, un * tg:un * (tg + 1)]
                            xf = (lambda v, rr=rr: v.re("p (u r) -> p r u", r=rr))
                        yield from norm_gen([rp.v()], 128, onesblk, 2, 1.0, [gsc[:, gi:gi + 1]], [outv], ssq_ps, sqr, sfr, xf=xf)
                        raw_ps.release(rp)

                    def vd_item(jp):
                        rp = raw_ps.next()
                        for k in range(8):
                            fw.matmul(rp[:, 0:256], hview(k, jp, 1), wvg[:, k, :], start=(k == 0), stop=(k == 7), signal=(k == 7))
                        yield
                        fw.copy("act", vg[:, jp, :, 0:64], rp[:, 0:256].re("p (h d) -> p h d", h=4))

                    def dprep_items():
                        for tg in range(4):
                            for (wsl, gi, dstb) in ((wqg, 2 + g, qg), (wkg, 5 + g, kg)):
                                for c in range(2):
                                    yield qk_item(tg, wsl, gi, dstb, c)
                            for t4 in range(4):
                                yield vd_item(tg * 4 + t4)
                    pipeline(dprep_items(), 4, warm=(PB[7], K_DP))
                    if g + 1 < 3:
                        dil_loads(g + 1)

                    s_ps = Rot(PB[0:4])
                    zbank = PB[4]
                    ubanks = Rot(PB[5:8])
                    bank = {}

                    def evac_bank(hl, n):
                        up = bank.pop((hl, n))
                        src = up[0:65, :]
                        if g == 0:
                            fw.copy("act", Ut[hl][0:65, n * 512:(n + 1) * 512], src)
                            return
                        if g == 1:
                            dst = Ut[hl][0:65, n:S:4]
                        else:
                            dst = View(Ut[hl], Ut[hl].ap[0:65, :].rearrange("p (i s) -> p s i", s=16)[:, 4 * n:4 * n + 4, :])
                            src = src.re("p (t i) -> p t i", t=4)
                        fw.tt("dve", dst, dst, src, ALU.add)

                    def dil_item(hl, pr):
                        c, r0 = hl // 2, (hl % 2) * 64
                        rows = slice(r0, r0 + 64)
                        tiles = []
                        off = 0
                        sp_ = s_ps.next()
                        for jp in (2 * pr, 2 * pr + 1):
                            b = jp % nb
                            N = 256 if b + 1 < nb else 128
                            tiles.append((jp, off, N))
                            fw.matmul(sp_[:, off:off + N], kg[rows, c, jp * 128:(jp + 1) * 128], qg[rows, c, jp * 128:jp * 128 + N])
                            off += N
                        yield
                        e = er.next()
                        fw.act(e[:, 0:off], sp_[:, 0:off], AF.Exp, scale=0.125)
                        s_ps.release(sp_)
                        yield
                        for (jp, o, N) in tiles:
                            fw.tt("dve", e[:, o:o + N], e[:, o:o + N], mtb[:, hl, 0:N], ALU.mult)
                        yield
                        for _ in range(K_DD):
                            fw.matmul(zbank[:, 0:512], ident, cM[:, 0:512], signal=False)
                        for (jp, o, N) in tiles:
                            targets = [(jp, e[:, o:o + 128])]
                            if N == 256:
                                targets.append((jp + 1, e[:, o + 128:o + 256]))
                            for (tj, rhs) in targets:
                                n = tj // 4
                                first = (hl, n) not in bank
                                if first:
                                    bank[(hl, n)] = ubanks.next()
                                up = bank[(hl, n)]
                                fw.matmul(up[0:65, (tj % 4) * 128:(tj % 4) * 128 + 128], vg[:, jp, hl, :], rhs,
                                          start=first, stop=True, signal=True, skip_group_check=True)
                            if jp % 4 == 3:
                                evac_bank(hl, jp // 4)
                    pipeline((dil_item(hl, pr) for hl in range(4) for pr in range(8)), 4)
                arD.reset(markU)
                utb = Rot([Buf(arD.alloc([512], F32), "rd%d" % i) for i in range(3)], "r18")
                b_ps = Rot(PB[0:4])

                def dn_item(hl, j):
                    cols = slice(j * 512, (j + 1) * 512)
                    bp = b_ps.next()
                    fw.matmul(bp[0:64, :], Ef.v(), Ut[hl][0:65, cols])
                    yield
                    rd = utb.next()
                    fw.act(rd[0:64, :], bp[0:64, :], AF.Ln)
                    yield
                    fw.act(rd[0:64, :], rd[0:64, :], AF.Exp, scale=-1.0)
                    yield
                    fw.tt("dve", odil[0:64, hh * 4 + hl, cols], Ut[hl][0:64, cols], rd[0:64, :], ALU.mult)
                pipeline((dn_item(hl, j) for hl in range(4) for j in range(4)), 3)
                if stop == 3:
                    fw.finish()
                    return nc, dbg_out

            arD.reset()
            mrg = Buf(arD.alloc([8, S], BF16), "mrg")
            mark_mrg = arD.off
            woa = Buf(arD.alloc([8, DM], BF16), "woa")
            wob = Buf(arD.alloc([8, DM], BF16), "wob")
            load("pool", woa[0:64, :, :], woa_d.rearrange("(h d) n -> d h n", d=64))
            load("pool", wob[0:64, :, :], wob_d.rearrange("(h d) n -> d h n", d=64))
            wgs = Rot([Buf(arD.alloc([8, 256], BF16), "wgs%d" % i) for i in range(2)], "r19")
            sgr = Rot([Buf(arD.alloc([512], F32), "sg%d" % i) for i in range(4)], "r20")
            m1r = Rot([Buf(arD.alloc([512], F32), "m1%d" % i) for i in range(5)], "r21")
            ps4 = Rot(PB[0:7])
            wgcur = {}

            m1A = {}

            def mg_item(c, tg, br):
                if tg == 0 and br == 0:
                    wgc = wgs.next()
                    wgs.release(wgc)
                    slab_load(wgc[:, :, 0:128], GATEB + c * 128, 128)
                    slab_load(wgc[:, :, 128:256], GATEB + 1024 + c * 128, 128)
                    wgcur[c] = wgc
                wgc = wgcur[c]
                cols = slice(tg * 512, (tg + 1) * 512)
                wo, osrc = ((woa, omla), (wob, odil))[br]
                gp = ps4.next()
                for k in range(8):
                    fw.matmul(gp.v(), wgc[:, k, br * 128:(br + 1) * 128], hT[:, k, cols], start=(k == 0), stop=(k == 7), signal=(k == 7))
                pp = ps4.next()
                for h in range(8):
                    fw.matmul(pp.v(), wo[0:64, h, c * 128:(c + 1) * 128], osrc[0:64, h, cols], start=(h == 0), stop=(h == 7), signal=(h == 7))
                yield
                sg = sgr.next()
                fw.act(sg.v(), gp.v(), AF.Sigmoid, bias=cA[:, C_BG + br * 8 + c:C_BG + br * 8 + c + 1])
                ps4.release(gp)
                yield
                m1 = m1r.next()
                fw.tt("dve", m1.v(), sg.v(), pp.v(), ALU.mult)
                ps4.release(pp)
                sgr.release(sg)
                if br == 0:
                    m1r.hold(m1)
                    m1A[(c, tg)] = m1
                    return
                yield
                ma = m1A.pop((c, tg))
                fw.tt("pool", mrg[:, c, cols], ma.v(), m1.v(), ALU.add)
                m1r.release(ma)

            pipeline((mg_item(c, tg, br) for c in range(8) for tg in range(4) for br in range(2)), 3, warm=(PB[7], K_D3))
            fw_barrier(fw)
            if stop == 4:
                fw.finish()
                return nc, dbg_out

            arAB.reset()
            arC.reset()
            x1 = [Buf(arAB.alloc([DM], F32), "x1") for _ in range(NT)]
            h2T = Buf(arC.alloc([8, S], BF16), "h2T")
            arD.reset(mark_mrg)
            wo_ = Buf(arD.alloc([8, DM], BF16), "wout")
            load("pool", wo_.v(), wout_d.rearrange("(c p) n -> p c n", p=128))
            junk = Buf(arD.alloc([DM], BF16), "junk")
            xbs = Rot([Buf(arD.alloc([DM], BF16), "xb%d" % i) for i in range(3)], "r22")
            sts = Rot([Buf(arD.alloc([4], F32), "st%d" % i) for i in range(4)], "r23")
            xts = Rot([Buf(arD.alloc([DM], F32), "xt%d" % i) for i in range(3)], "r24")
            y_ps = Rot(PW[0:3], "p3y")
            t_ps = Rot(PB[6:7], "p3t")
            l_ps = Rot(PB[7:8], "p3l")
            gF_bc = View(cA, cA.ap[:, C_GF:C_GF + 8].unsqueeze(2).broadcast_to([128, 8, 128]))

            def p3_item(t):
                tc_ = slice(t * 128, (t + 1) * 128)
                xt = xts.next()
                load("sp", xt.v(), x_d[s, tc_, :])
                yp = y_ps.next()
                for half in range(2):
                    for k in range(8):
                        fw.matmul(yp[:, half * 512:(half + 1) * 512], mrg[:, k, tc_], wo_[:, k, half * 512:(half + 1) * 512],
                                  start=(k == 0), stop=(k == 7), signal=(k == 7 and half == 1))
                yield
                fw.tt("dve", x1[t].v(), yp.v(), xt.v(), ALU.add)
                y_ps.release(yp)
                yield
                stt_ = sts.next()
                fw.act(junk.v(), x1[t].v(), AF.Square, accum_out=stt_[:, 0:1])
                yield
                fw.act(stt_[:, 1:2], stt_[:, 0:1], AF.Sqrt, scale=1.0 / DM, bias=epsc[:, 0:1])
                yield
                fw.recip(stt_[:, 2:3], stt_[:, 1:2])
                yield
                xb = xbs.next()
                fw.tscalar("dve", xb.v(), x1[t].v(), stt_[:, 2:3], None, ALU.mult)
                yield
                pb = t_ps.next()
                pbv = View(pb, shaped(pb.ap.bitcast(BF16), [8, 128]))
                for k in range(8):
                    fw.transpose(pbv[:, k, :], xb[:, k * 128:(k + 1) * 128], ident, signal=(k == 7))
                yield
                fw.tt("dve", h2T[:, :, tc_], pbv, gF_bc, ALU.mult)
                t_ps.release(pb)
                yield
                lp = l_ps.next()
                for k in range(8):
                    fw.matmul(lp[:, 0:36], h2T[:, k, tc_], wrb[:, k, :], start=(k == 0), stop=(k == 7), signal=(k == 7))
                yield
                fw.tt("dve", LG[:, t, :], lp[:, 0:36], cA[:, C_BR:C_BR + 36], ALU.add)
            pipeline((p3_item(t) for t in range(NT)), 3)

            def bc3(v, n):
                return View(v.buf, v.ap.unsqueeze(2).broadcast_to([128, NT, n]))
            gmx, gsum, gp_, m1_, m2_, dd, w1, w2 = [RS[:, :, i] for i in range(8)]
            ohg = RS[:, :, 8:12]
            eg = RS[:, :, 12:16]
            els = RS[:, :, 16:24]
            oh1 = RS[:, :, 24:32]
            msk = RS[:, :, 32:40]
            oh2 = RS[:, :, 40:48]
            tmp8 = RS[:, :, 48:56]
            fw.reduce("dve", gmx, LG[:, :, 0:4], ALU.max)
            fw.tt("dve", ohg, LG[:, :, 0:4], bc3(gmx, 4), ALU.is_equal)
            fw.tt("dve", eg, LG[:, :, 0:4], bc3(gmx, 4), ALU.subtract)
            fw.act(eg, eg, AF.Exp)
            fw.reduce("dve", gsum, eg, ALU.add)
            fw.recip(gp_, gsum)
            for gi in range(4):
                dstv = els if gi == 0 else tmp8
                fw.tt("dve", dstv, LG[:, :, 4 + gi * 8:12 + gi * 8], bc3(ohg[:, :, gi], 8), ALU.mult)
                if gi > 0:
                    fw.tt("dve", els, els, tmp8, ALU.add)
            fw.reduce("dve", m1_, els, ALU.max)
            fw.tt("dve", oh1, els, bc3(m1_, 8), ALU.is_equal)
            fw.stt("dve", msk, oh1, -1e30, els, ALU.mult, ALU.add)
            fw.reduce("dve", m2_, msk, ALU.max)
            fw.tt("dve", oh2, msk, bc3(m2_, 8), ALU.is_equal)
            fw.tt("dve", dd, m2_, m1_, ALU.subtract)
            fw.act(dd, dd, AF.Exp)
            fw.tscalar("dve", w2, dd, 1.0, None, ALU.add)
            fw.recip(w2, w2)
            fw.tt("dve", w1, w2, gp_, ALU.mult)
            fw.tt("dve", w2, w1, dd, ALU.mult)
            fw.tt("dve", oh1, oh1, bc3(w1, 8), ALU.mult)
            fw.tt("dve", oh2, oh2, bc3(w2, 8), ALU.mult)
            fw.tt("dve", oh1, oh1, oh2, ALU.add)
            for gi in range(4):
                fw.tt("dve", comb[:, :, gi * 8:(gi + 1) * 8], oh1, bc3(ohg[:, :, gi], 8), ALU.mult)
            fw_barrier(fw)
            if stop == 5:
                fw.finish()
                return nc, dbg_out

            arD.reset()
            slots = Rot([Buf(arD.alloc([3, 8, 256], BF16), "ex%d" % i) for i in range(3)], "r25")
            sgr = Rot([Buf(arD.alloc([512], F32), "sg%d" % i) for i in range(2)], "r26")
            her = Rot([Buf(arD.alloc([2, 512], BF16), "he%d" % i) for i in range(3)], "r27")
            gu_ps = Rot(PB[0:4])
            y_ps = Rot(PW[2:4])

            def ex_load(e):
                sl = slots.next()
                load("pool", sl[:, 0, :, :], wg_d[e].rearrange("(c p) n -> p c n", p=128))
                load("pool", sl[:, 1, :, :], wu_d[e].rearrange("(c p) n -> p c n", p=128))
                load("pool", sl[:, 2, :, :].re("p (c x) n -> p c (x n)", c=2), wd_d[e].rearrange("(c p) n -> p c n", p=128))
                return sl
            exq = [ex_load(0), ex_load(1)]
            msteps = [(e, tg) for e in range(32) for tg in range(4)]
            mstate = {}

            def moe_front_half(st_, ffc):
                e, tg = st_
                if tg == 1 and ffc == 0:
                    if e + 2 < 32:
                        exq.append(ex_load(e + 2))
                sl = exq[e]
                cols = slice(tg * 512, (tg + 1) * 512)
                if ffc == 0:
                    mstate[st_] = her.next()
                he = mstate[st_]
                gp = gu_ps.next()
                up = gu_ps.next()
                for k in range(8):
                    fw.matmul(gp.v(), sl[:, 0, k, ffc * 128:(ffc + 1) * 128], h2T[:, k, cols], start=(k == 0), stop=(k == 7), signal=(k == 7))
                for k in range(8):
                    fw.matmul(up.v(), sl[:, 1, k, ffc * 128:(ffc + 1) * 128], h2T[:, k, cols], start=(k == 0), stop=(k == 7), signal=(k == 7))
                sg = sgr.next()
                fw.act(sg.v(), gp.v(), AF.Silu)
                fw.tt("dve", he[:, ffc, :], sg.v(), up.v(), ALU.mult)

            def moe_back_half(st_, hf):
                e, tg = st_
                sl = exq[e]
                he = mstate[st_]
                wdv = sl[:, 2, :, :].re("p (c x) n -> p c (x n)", c=2)
                for t4 in (2 * hf, 2 * hf + 1):
                    t = tg * 4 + t4
                    yp = y_ps.next()
                    for half in range(2):
                        for ffc in range(2):
                            fw.matmul(yp[:, half * 512:(half + 1) * 512], he[:, ffc, t4 * 128:(t4 + 1) * 128],
                                      wdv[:, ffc, half * 512:(half + 1) * 512], start=(ffc == 0), stop=(ffc == 1),
                                      signal=(ffc == 1 and half == 1))
                    fw.stt("dve", x1[t].v(), yp.v(), comb[:, t, e:e + 1], x1[t].v(), ALU.mult, ALU.add)
                    if e == 31:
                        fw.dma("sp", y_d[s, t * 128:(t + 1) * 128, :], x1[t].v(), out_is_dram=True, sem=ssem)
                if hf == 1:
                    mstate.pop(st_)

            for i in range(len(msteps) + 1):
                for hf in range(2):
                    if i < len(msteps):
                        moe_front_half(msteps[i], hf)
                    if i - 1 >= 0:
                        moe_back_half(msteps[i - 1], hf)
            fw_barrier(fw)
        fw.finish()
    return nc, dbg_out


def _consts():
    cm = np.zeros((128, NCM), np.float32)
    cm[:, M_ID:M_ID + 128] = np.eye(128, dtype=np.float32)
    cm[:, M_ONES:M_ONES + 128] = 1.0
    blk = np.zeros((128, 128), np.float32)
    blk[:64, :64] = 1.0
    blk[64:, 64:] = 1.0
    cm[:, M_BLK:M_BLK + 128] = blk
    cm[:96, M_O96:M_O96 + 96] = 1.0
    pt = np.zeros((96, 96), np.float32)
    for i in range(16):
        pt[80 + i, 64 + i] = -1.0
        pt[64 + i, 80 + i] = 1.0
    cm[:96, M_PT:M_PT + 96] = pt
    ish = np.zeros((96, 96), np.float32)
    for i in range(64, 96):
        ish[i, i] = 1.0
    cm[:96, M_ISH:M_ISH + 96] = ish
    p = np.arange(128)[:, None]
    f = np.arange(128)[None, :]
    cm[:, M_CAUS:M_CAUS + 128] = (f >= p).astype(np.float32)
    ce = np.zeros((65, 64), np.float32)
    ce[64, :] = 1.0
    mt = np.zeros((3, 128, 8, 256), np.float32)
    slopes = np.exp2(-8.0 * (np.arange(8, dtype=np.float32) + 1.0) / 8.0).astype(np.float32)
    for g, dil in enumerate(DILS):
        for h in range(8):
            dcur = (f - p).astype(np.float32)
            cur = np.where(f >= p, np.exp(-slopes[h] * dcur * dil), 0.0)
            dprev = (128 + f - p).astype(np.float32)
            prev = np.where(f <= p, np.exp(-slopes[h] * dprev * dil), 0.0)
            mt[g, :, h, 0:128] = cur
            mt[g, :, h, 128:256] = prev
    freq = (10000.0 ** (-np.arange(16, dtype=np.float32) / 16.0)).astype(np.float32)
    return cm, ce, mt, freq


def kernel(x, positions, norm_attn, w_in, b_gate, norm_ckv, w_ukv, q_norm_mla, k_norm_mla,
           q_norm_dil, k_norm_dil, w_o_mla, w_o_dil, w_out, norm_ffn, w_router_group,
           b_router_group, w_router_expert, b_router_expert, w_gate, w_up, w_down):
    f = lambda a: np.ascontiguousarray(np.asarray(a), dtype=np.float32)
    x = f(x)
    positions = np.ascontiguousarray(np.asarray(positions), dtype=np.int32)
    ncores = 8
    nseq = x.shape[0] // ncores
    cm, ce, mt, freq = _consts()
    ca = np.zeros((128, NCA), np.float32)
    ca[:, C_GA:C_GA + 8] = f(norm_attn)[0].reshape(8, 128).T
    ca[:, C_GF:C_GF + 8] = f(norm_ffn)[0].reshape(8, 128).T
    ca[:, C_GC:C_GC + 2] = f(norm_ckv)[0].reshape(2, 128).T
    ca[:, C_BG:C_BG + 16] = f(b_gate)[0].reshape(16, 128).T
    ca[:96, C_GQM] = f(q_norm_mla)[0]
    ca[:96, C_GKM] = f(k_norm_mla)[0]
    ca[:, C_GQD:C_GQD + 3] = np.tile(f(q_norm_dil)[0], (1, 2)).T
    ca[:, C_GKD:C_GKD + 3] = np.tile(f(k_norm_dil)[0], (1, 2)).T
    ca[64:80, C_FRQ] = freq
    ca[80:96, C_FRQ] = freq
    ca[:, C_BR:C_BR + 4] = f(b_router_group)[0][None, :]
    ca[:, C_BR + 4:C_BR + 36] = f(b_router_expert)[0][None, :]
    wr = np.ascontiguousarray(np.concatenate([f(w_router_group)[0], f(w_router_expert)[0]], axis=1))
    shared = {
        "w_in": f(w_in)[0], "w_ukv": f(w_ukv)[0], "w_o_mla": f(w_o_mla)[0], "w_o_dil": f(w_o_dil)[0],
        "w_out": f(w_out)[0], "w_gate": f(w_gate)[0], "w_up": f(w_up)[0], "w_down": f(w_down)[0],
        "wr": wr, "cst_a": ca, "cst_m": cm, "cst_e": ce, "mtab": mt,
    }
    nc, _ = build_nc(nseq)
    in_maps = []
    for c in range(ncores):
        m = dict(shared)
        m["x"] = np.ascontiguousarray(x[c * nseq:(c + 1) * nseq])
        m["pos"] = np.ascontiguousarray(positions[c * nseq:(c + 1) * nseq])
        in_maps.append(m)
    res = run_bass_kernel_spmd(nc, in_maps, core_ids=list(range(ncores)))
    out = np.concatenate([np.asarray(r["y"]) for r in res.results], axis=0)
    return out.astype(np.float32)
```

```python
import numpy as np
import concourse.bass as bass
import concourse.mybir as mybir

F32 = mybir.dt.float32
BF16 = mybir.dt.bfloat16
I32 = mybir.dt.int32
ALU = mybir.AluOpType
AF = mybir.ActivationFunctionType
AX = mybir.AxisListType


LAST_ALLOC = [None]
REG = []


class Sem:
    def __init__(self, h, name):
        self.h = h
        self.name = name
        self.count = 0


class Buf:
    def __init__(self, ap, name=""):
        self.ap = ap
        self.name = name
        self.w = None
        self.r = {}
        self.dsem = None
        if LAST_ALLOC[0] is not None:
            st, n = LAST_ALLOC[0]
            LAST_ALLOC[0] = None
            merged = {}
            keep = []
            for (s0, e0, ob) in REG:
                if s0 < st + n and st < e0:
                    evs = list(ob.r.items())
                    if ob.w is not None:
                        evs.append(ob.w)
                    for sem, val in evs:
                        if merged.get(sem, 0) < val:
                            merged[sem] = val
                else:
                    keep.append((s0, e0, ob))
            keep.append((st, st + n, self))
            REG[:] = keep
            self.r = merged

    def __getitem__(self, idx):
        return View(self, self.ap[idx])

    def v(self, ap=None):
        return View(self, self.ap if ap is None else ap)


class View:
    def __init__(self, buf, ap):
        self.buf = buf
        self.ap = ap

    def __getitem__(self, idx):
        return View(self.buf, self.ap[idx])

    def re(self, s, **kw):
        return View(self.buf, self.ap.rearrange(s, **kw))

    def bc(self, shape):
        return View(self.buf, self.ap.broadcast_to(shape))


class Eng:
    def __init__(self, name, sem):
        self.name = name
        self.sem = sem
        self.ops = []
        self.known = {}
        self.pending = False


class FW:
    def __init__(self, nc, stack):
        self.nc = nc
        self.stack = stack
        self.nsem = 0
        self.E = {}
        for n in ("pe", "act", "dve", "pool", "sp"):
            self.E[n] = Eng(n, self.new_sem("s_" + n))
        self.out_events = []

    def new_sem(self, name):
        h = self.stack.enter_context(self.nc.semaphore(name))
        self.nsem += 1
        return Sem(h, name)

    def sbuf(self, name, shape, dtype):
        t = self.stack.enter_context(self.nc.sbuf_tensor(name, list(shape), dtype))
        return t

    def psum(self, name, shape, dtype):
        t = self.stack.enter_context(self.nc.psum_tensor(name, list(shape), dtype))
        return t

    def _waits(self, E, reads, writes):
        need = {}

        def add(ev, same_engine_ok):
            if ev is None:
                return
            sem, val = ev
            if sem is E.sem and same_engine_ok:
                return
            if need.get(sem, 0) < val:
                need[sem] = val

        for v in reads:
            add(v.buf.w, False)
        for v in writes:
            add(v.buf.w, True)
            for sem, val in v.buf.r.items():
                add((sem, val), True)
        out = []
        for sem, val in need.items():
            if E.known.get(sem, 0) >= val:
                continue
            E.known[sem] = val
            out.append((sem, val))
        return out

    def _record(self, ev, reads, writes):
        sem, val = ev
        for v in reads:
            b = v.buf
            if b.r.get(sem, 0) < val:
                b.r[sem] = val
        for v in writes:
            b = v.buf
            b.w = ev
            b.r = {}

    def op(self, eng, make, reads=(), writes=(), signal=True):
        E = self.E[eng]
        waits = self._waits(E, reads, writes)
        val = E.sem.count + 1
        if signal:
            E.sem.count = val
            E.pending = False
        else:
            E.pending = True
        semh = E.sem.h

        def run(e, waits=waits, make=make, signal=signal, semh=semh):
            for s, v in waits:
                e.wait_ge(s.h, v)
            ins = make(e)
            if signal:
                ins.then_inc(semh, 1)

        E.ops.append(run)
        self._record((E.sem, val), reads, writes)

    def dma(self, q, out, in_, out_is_dram=False, in_is_dram=False, sem=None, **kw):
        E = self.E[q]
        reads = [] if in_is_dram else [in_]
        writes = [] if out_is_dram else [out]
        waits = self._waits(E, reads, writes)
        if sem is None:
            b = (writes[0] if writes else reads[0]).buf
            if b.dsem is None:
                b.dsem = self.new_sem("d_" + b.name)
            sem = b.dsem
        sem.count += 16
        val = sem.count
        oap = out if out_is_dram else out.ap
        iap = in_ if in_is_dram else in_.ap

        def run(e, waits=waits, oap=oap, iap=iap, semh=sem.h, kw=kw):
            for s, v in waits:
                e.wait_ge(s.h, v)
            e.dma_start(out=oap, in_=iap, **kw).then_inc(semh, 16)

        E.ops.append(run)
        self._record((sem, val), reads, writes)
        if out_is_dram:
            self.out_events.append((sem, val))
        return (sem, val)

    def finish(self):
        E = self.E["sp"]
        final = {}
        for sem, val in self.out_events:
            final[sem] = max(final.get(sem, 0), val)
        fl = list(final.items())

        def run(e, fl=fl):
            for s, v in fl:
                e.wait_ge(s.h, v)

        E.ops.append(run)
        nc = self.nc
        with nc.Block() as block:
            @block.tensor
            def _(e):
                for f in self.E["pe"].ops:
                    f(e)

            @block.scalar
            def _(e):
                for f in self.E["act"].ops:
                    f(e)

            @block.vector
            def _(e):
                for f in self.E["dve"].ops:
                    f(e)

            @block.gpsimd
            def _(e):
                for f in self.E["pool"].ops:
                    f(e)

            @block.sync
            def _(e):
                for f in self.E["sp"].ops:
                    f(e)

    def matmul(self, out, lhsT, rhs, start=True, stop=True, signal=True, extra_reads=(), **kw):
        self.op("pe", lambda e: e.matmul(out.ap, lhsT.ap, rhs.ap, start=start, stop=stop, **kw),
                reads=[lhsT, rhs, *extra_reads], writes=[out], signal=signal)

    def transpose(self, out, in_, ident, signal=True):
        self.op("pe", lambda e: e.transpose(out.ap, in_.ap, ident.ap),
                reads=[in_, ident], writes=[out], signal=signal)

    def act(self, out, in_, func, bias=None, scale=1.0, accum_out=None, eng="act"):
        reads = [in_]
        kw = {}
        if bias is not None:
            if isinstance(bias, View):
                reads.append(bias)
                kw["bias"] = bias.ap
            else:
                kw["bias"] = bias
        if isinstance(scale, View):
            reads.append(scale)
            sc = scale.ap
        else:
            sc = scale
        writes = [out]
        if accum_out is not None:
            writes.append(accum_out)
            kw["accum_out"] = accum_out.ap
        self.op("act", lambda e: e.activation(out.ap, in_.ap, func, scale=sc, **kw),
                reads=reads, writes=writes)

    def tscalar(self, eng, out, in0, s1, s2, op0, op1=None, accum_out=None):
        reads = [in0]
        a1 = s1
        a2 = s2
        if isinstance(s1, View):
            reads.append(s1)
            a1 = s1.ap
        if isinstance(s2, View):
            reads.append(s2)
            a2 = s2.ap
        kw = {}
        writes = [out]
        if op1 is not None:
            kw["op1"] = op1
        if accum_out is not None:
            kw["accum_out"] = accum_out.ap
            writes.append(accum_out)
        self.op(eng, lambda e: e.tensor_scalar(out.ap, in0.ap, a1, a2, op0, **kw),
                reads=reads, writes=writes)

    def tt(self, eng, out, in0, in1, op):
        self.op(eng, lambda e: e.tensor_tensor(out.ap, in0.ap, in1.ap, op),
                reads=[in0, in1], writes=[out])

    def stt(self, eng, out, in0, scalar, in1, op0, op1):
        reads = [in0, in1]
        sc = scalar
        if isinstance(scalar, View):
            reads.append(scalar)
            sc = scalar.ap
        self.op(eng, lambda e: e.scalar_tensor_tensor(out.ap, in0.ap, sc, in1.ap, op0, op1),
                reads=reads, writes=[out])

    def copy(self, eng, out, in_):
        if eng == "act":
            self.op("act", lambda e: e.copy(out.ap, in_.ap), reads=[in_], writes=[out])
        else:
            self.op(eng, lambda e: e.tensor_copy(out.ap, in_.ap), reads=[in_], writes=[out])

    def memset(self, eng, out, val):
        self.op(eng, lambda e: e.memset(out.ap, val), reads=[], writes=[out])

    def reduce(self, eng, out, in_, op, axis=None):
        axis = AX.X if axis is None else axis
        self.op(eng, lambda e: e.tensor_reduce(out.ap, in_.ap, axis, op), reads=[in_], writes=[out])

    def recip(self, out, in_):
        self.op("dve", lambda e: e.reciprocal(out.ap, in_.ap), reads=[in_], writes=[out])


import math
from contextlib import ExitStack
from concourse.bass_utils import run_bass_kernel_spmd

S = 2048
DM = 1024
NT = 16
EPS = 1e-6
DILB = 1056
GATEB = 1056 + 4608
DILS = (1, 4, 16)
NBLK = (16, 4, 1)
U8 = mybir.dt.uint8

C_GA, C_GF, C_GC, C_BG, C_GQM, C_GKM, C_GQD, C_GKD, C_FRQ, C_BR = 0, 8, 16, 18, 34, 35, 36, 39, 42, 43
NCA = 79
M_ID, M_ONES, M_BLK, M_O96, M_PT, M_ISH, M_CAUS = 0, 128, 256, 384, 480, 576, 672
NCM = 800


def _sz(dt):
    return {F32: 4, BF16: 2, I32: 4}[dt]


def shaped(ap, shape):
    if len(shape) == 1:
        return ap
    names = "abcd"[:len(shape)]
    pat = "p (" + " ".join(names) + ") -> p " + " ".join(names)
    kw = {n: s for n, s in zip(names[:-1], shape[:-1])}
    return ap.rearrange(pat, **kw)


class Arena:
    def __init__(self, t, base, size):
        self.t, self.base, self.off, self.end = t, base, base, base + size

    def reset(self, to=None):
        self.off = self.base if to is None else to

    def alloc(self, shape, dt):
        n = _sz(dt)
        for s in shape:
            n *= s
        n = (n + 63) // 64 * 64
        assert self.off + n <= self.end, ("arena overflow", self.off, n, self.end)
        ap = self.t[:, self.off:self.off + n]
        self.off += n
        nb = _sz(dt)
        tot = 1
        for s in shape:
            tot *= s
        ap = self.t[:, self.off - n:self.off - n + tot * nb].bitcast(dt)
        LAST_ALLOC[0] = (self.off - n, n)
        return shaped(ap, shape)


ACTIVE = {"HOLD"}
CUR = [None]
TOKC = [0]


class Rot:
    def __init__(self, bufs, name="rot"):
        self.bufs = bufs
        self.i = 0
        self.busy = [None] * len(bufs)
        self.name = name

    def next(self):
        n = len(self.bufs)
        for k in range(n):
            j = (self.i + k) % n
            o = self.busy[j]
            if o is None or o not in ACTIVE:
                self.busy[j] = CUR[0]
                self.i = j + 1
                return self.bufs[j]
        raise RuntimeError("rotation too shallow: %s (size %d)" % (self.name, n))

    def release(self, buf):
        for j, b in enumerate(self.bufs):
            if b is buf:
                self.busy[j] = None

    def hold(self, buf):
        for j, b in enumerate(self.bufs):
            if b is buf:
                self.busy[j] = "HOLD"


def fw_barrier(fw):
    sems = fw.sems
    for E in fw.E.values():
        assert not E.pending, E.name
        waits = []
        for s in sems:
            if s is E.sem:
                continue
            if s.count > E.known.get(s, 0):
                E.known[s] = s.count
                waits.append((s, s.count))

        def run(e, waits=waits):
            for s, v in waits:
                e.wait_ge(s.h, v)
        E.ops.append(run)


import os
PIPE_MAX = int(os.environ.get('K_PIPE', '8'))
K_LN = os.environ.get('K_LN', '1') == '1'


def build_nc(NSEQ=2, dbg=None, stop=None):
    nc = bass.Bass("TRN2", target_bir_lowering=False)
    dr = lambda name, shape, dt=F32: nc.dram_tensor(name, list(shape), dt, kind="ExternalInput").ap()
    x_d = dr("x", [NSEQ, S, DM])
    pos_d = dr("pos", [NSEQ, S], I32)
    win_d = dr("w_in", [DM, 7712])
    wukv_d = dr("w_ukv", [256, 1024])
    woa_d = dr("w_o_mla", [512, 1024])
    wob_d = dr("w_o_dil", [512, 1024])
    wout_d = dr("w_out", [DM, DM])
    wg_d = dr("w_gate", [32, DM, 256])
    wu_d = dr("w_up", [32, DM, 256])
    wd_d = dr("w_down", [32, 256, DM])
    wr_d = dr("wr", [DM, 36])
    ca_d = dr("cst_a", [128, NCA])
    cm_d = dr("cst_m", [128, NCM])
    e_d = dr("cst_e", [65, 64])
    mt_d = dr("mtab", [3, 128, 8, 256])
    y_d = nc.dram_tensor("y", [NSEQ, S, DM], F32, kind="ExternalOutput").ap()
    dbg_out = {}

    with ExitStack() as st:
        fw = FW(nc, st)
        fw.sems = [e.sem for e in fw.E.values()]
        csem = fw.new_sem("csem")
        ssem = fw.new_sem("ssem")
        def newc():
            sm = fw.new_sem("cs%d" % fw.nsem)
            fw.sems.append(sm)
            return sm
        lsem = {"pool": [fw.new_sem("lp%d" % i) for i in range(14)], "sp": [fw.new_sem("ls%d" % i) for i in range(6)]}
        fw.sems += [csem, ssem] + lsem["pool"] + lsem["sp"]
        lidx = {"pool": 0, "sp": 0}

        def load(q, out, in_, **kw):
            s = lsem[q][lidx[q] % len(lsem[q])]
            lidx[q] += 1
            fw.dma(q, out, in_, in_is_dram=True, sem=s, **kw)

        def pipeline(gens, depth):
            depth = min(depth, PIPE_MAX)
            active = []
            tok = {}
            it = iter(gens)
            done = False
            while True:
                for g_ in list(active):
                    CUR[0] = tok[id(g_)]
                    try:
                        next(g_)
                    except StopIteration:
                        active.remove(g_)
                        ACTIVE.discard(tok.pop(id(g_)))
                if not done and len(active) < depth:
                    try:
                        g_ = next(it)
                        TOKC[0] += 1
                        tok[id(g_)] = TOKC[0]
                        CUR[0] = TOKC[0]
                        ACTIVE.add(TOKC[0])
                        try:
                            next(g_)
                            active.append(g_)
                        except StopIteration:
                            ACTIVE.discard(tok.pop(id(g_)))
                    except StopIteration:
                        done = True
                CUR[0] = None
                if done and not active:
                    break

        def pers(name, shape, dt):
            LAST_ALLOC[0] = None
            return Buf(fw.sbuf(name, shape, dt)[:], name)
        cA = pers("cA", [128, NCA], F32)
        cM = pers("cM", [128, NCM], BF16)
        Ef = pers("Ef", [65, 64], F32)
        Wkr = pers("Wkr", [128, 8, 96], BF16)
        wkk = pers("wkk", [128, 2, 8, 96], BF16)
        wkv = pers("wkv", [128, 2, 8, 64], BF16)
        wrb = pers("wrb", [128, 8, 36], BF16)
        epsc = pers("epsc", [128, 4], F32)
        gsc = pers("gsc", [128, 8], F32)
        comb = pers("comb", [128, NT, 32], F32)
        LG = pers("LG", [128, NT, 36], F32)
        RS = pers("RS", [128, 16, 64], F32)
        arena_t = fw.sbuf("arena", [128, 190464], U8)
        A_OFF, B_OFF, C_OFF, D_OFF = 0, 32768, 65536, 98304
        arD = Arena(arena_t, D_OFF, 92160)
        arB = Arena(arena_t, B_OFF, 32768)
        arC = Arena(arena_t, C_OFF, 32768)
        arAB = Arena(arena_t, A_OFF, 65536)
        pst = [fw.psum("ps%d" % i, [128, 1024], F32) for i in range(4)]
        LAST_ALLOC[0] = None
        PB = [Buf(pst[i // 2][:, (i % 2) * 512:(i % 2) * 512 + 512], "pb%d" % i) for i in range(8)]
        PW = [Buf(pst[i][:], "pw%d" % i) for i in range(4)]

        fw.dma("sp", cA.v(), ca_d, in_is_dram=True, sem=newc())
        fw.dma("sp", Ef.v(), e_d, in_is_dram=True, sem=newc())
        fw.dma("pool", cM.v(), cm_d, in_is_dram=True, sem=newc())
        fw.memset("dve", Wkr.v(), 0.0)
        fw.memset("dve", wkk.v(), 0.0)
        fw.dma("pool", Wkr[:, :, 64:96], win_d[:, 1024:1056].rearrange("(c p) n -> p c n", p=128), in_is_dram=True, sem=newc())
        ukv = wukv_d.rearrange("(c p) (h d) -> p c h d", p=128, d=128)
        for c in range(2):
            fw.dma("pool", wkk[:, c, :, 0:64], ukv[:, c, :, 0:64], in_is_dram=True, sem=newc())
            fw.dma("pool", wkv[:, c, :, :], ukv[:, c, :, 64:128], in_is_dram=True, sem=newc())
        fw.dma("pool", wrb.v(), wr_d.rearrange("(c p) n -> p c n", p=128), in_is_dram=True, sem=newc())
        fw.memset("dve", epsc[:, 0:1], EPS)
        fw.memset("dve", epsc[:, 1:2], 96 * EPS)
        fw.memset("dve", epsc[:, 2:3], 64 * EPS)
        fw.memset("dve", epsc[:, 3:4], 0.0)
        fw_barrier(fw)
        fw.tscalar("dve", gsc[:, 0:2], cA[:, C_GQM:C_GQM + 2], math.sqrt(96.0), None, ALU.mult)
        fw.tscalar("dve", gsc[:, 2:8], cA[:, C_GQD:C_GQD + 6], 8.0, None, ALU.mult)
        fw_barrier(fw)

        ident = cM[:, M_ID:M_ID + 128]
        ones128 = cM[:, M_ONES:M_ONES + 128]
        onesblk = cM[:, M_BLK:M_BLK + 128]
        ones96 = cM[0:96, M_O96:M_O96 + 96]
        PTm = cM[0:96, M_PT:M_PT + 96]
        ISH = cM[0:96, M_ISH:M_ISH + 96]
        caus = cM[:, M_CAUS:M_CAUS + 128]

        def slab_load(dst, c0, ncols):
            load("pool", dst, win_d[:, c0:c0 + ncols].rearrange("(c p) n -> p c n", p=128))


        def norm_gen(raws, nrows, ones_l, eps_col, inv_n, gcols, outs, ssq_rot, sq_rot, sf_rot, xf=None):
            n = len(raws)
            qs = []
            for i, r in enumerate(raws):
                q = sq_rot.next()
                fw.act(q[0:nrows, :], r, AF.Square)
                qs.append(q)
            yield
            ssq = ssq_rot.next()
            for i, q in enumerate(qs):
                fw.matmul(ssq[0:nrows, :], ones_l, q[0:nrows, :], start=(i == 0), stop=(i == n - 1), signal=(i == n - 1))
            yield
            sv = sf_rot.next()
            if K_LN:
                fw.act(sv[0:nrows, :], ssq[0:nrows, :], AF.Ln, scale=inv_n, bias=epsc[0:nrows, eps_col:eps_col + 1])
                ssq_rot.release(ssq)
                fw.act(sv[0:nrows, :], sv[0:nrows, :], AF.Exp, scale=-0.5)
            else:
                fw.act(sv[0:nrows, :], ssq[0:nrows, :], AF.Sqrt, scale=inv_n, bias=epsc[0:nrows, eps_col:eps_col + 1])
                ssq_rot.release(ssq)
                yield
                fw.recip(sv[0:nrows, :], sv[0:nrows, :])
            yield
            for r, g, o in zip(raws, gcols, outs):
                if xf is None:
                    fw.stt("dve", o, r, g, sv[0:nrows, :], ALU.mult, ALU.mult)
                else:
                    fw.stt("dve", o, xf(r), g, xf(sv[0:nrows, :]), ALU.mult, ALU.mult)

        for s in range(NSEQ):
            LAST_ALLOC[0] = (A_OFF, 32768)
            hT = Buf(shaped(arena_t[:, A_OFF:A_OFF + 32768].bitcast(BF16), [8, S]), "hT")
            arD.reset()
            xts = Rot([Buf(arD.alloc([DM], F32), "xt%d" % i) for i in range(4)], "r1")
            junk = Buf(arD.alloc([DM], BF16), "junk")
            xbs = Rot([Buf(arD.alloc([DM], BF16), "xb%d" % i) for i in range(4)], "r2")
            sts = Rot([Buf(arD.alloc([4], F32), "st%d" % i) for i in range(4)], "r3")
            t_ps = Rot(PB[0:4])
            gA_bc = View(cA, cA.ap[:, C_GA:C_GA + 8].unsqueeze(2).broadcast_to([128, 8, 128]))

            def p0_item(t):
                xt = xts.next()
                load("sp", xt.v(), x_d[s, t * 128:(t + 1) * 128, :])
                stt_ = sts.next()
                fw.act(junk.v(), xt.v(), AF.Square, accum_out=stt_[:, 0:1])
                yield
                fw.act(stt_[:, 1:2], stt_[:, 0:1], AF.Sqrt, scale=1.0 / DM, bias=epsc[:, 0:1])
                yield
                fw.recip(stt_[:, 2:3], stt_[:, 1:2])
                yield
                xb = xbs.next()
                fw.tscalar("dve", xb.v(), xt.v(), stt_[:, 2:3], None, ALU.mult)
                yield
                pb = t_ps.next()
                pbv = View(pb, shaped(pb.ap.bitcast(BF16), [8, 128]))
                for k in range(8):
                    fw.transpose(pbv[:, k, :], xb[:, k * 128:(k + 1) * 128], ident, signal=(k == 7))
                yield
                fw.tt("dve", hT[:, :, t * 128:(t + 1) * 128], pbv, gA_bc, ALU.mult)
            pipeline((p0_item(t) for t in range(NT)), 4)
            if stop == 0:
                fw.finish()
                return nc, dbg_out

            LAST_ALLOC[0] = (B_OFF, 32768)
            omla = Buf(shaped(arena_t[:, B_OFF:B_OFF + 32768].bitcast(BF16), [8, S]), "omla")
            arD.reset()
            arC.reset()
            qT = [Buf(arD.alloc([S], BF16), "qT") for _ in range(8)]
            kT = [Buf(arD.alloc([S], BF16), "kT") for _ in range(8)]
            vaug = Buf(arC.alloc([NT, 8, 65], BF16), "vaug")
            wq = Buf(arC.alloc([8, 768], BF16), "wq")
            markD = arD.off
            wc = Buf(arD.alloc([8, 256], BF16), "wc")
            slab_load(wq.v(), 0, 768)
            slab_load(wc.v(), 768, 256)
            fw.memset("pool", vaug[:, :, :, 64:65], 1.0)
            ckvn_r = Rot([Buf(arD.alloc([2, 512], BF16), "ckvn%d" % i) for i in range(2)], "r4")
            krs_r = Rot([Buf(arD.alloc([512], BF16), "krs%d" % i) for i in range(2)], "r5")
            arB.reset()
            sqr = Rot([Buf(arB.alloc([512], BF16), "sq%d" % i) for i in range(4)], "r6")
            sfr = Rot([Buf(arB.alloc([512], F32), "sf%d" % i) for i in range(3)], "r7")
            ropeC_r = Rot([Buf(arB.alloc([512], F32), "ropeC%d" % i) for i in range(2)], "r8")
            ropeS_r = Rot([Buf(arB.alloc([512], F32), "ropeS%d" % i) for i in range(2)], "r9")
            posi = Buf(arB.alloc([512], I32), "posi")
            angb = Buf(arB.alloc([512], F32), "ang")
            tqb = Buf(arB.alloc([512], F32), "tq")
            kib = posi
            kfb = Buf(arB.alloc([512], F32), "kf")
            rtmp = Rot([Buf(arD.alloc([512], F32), "rt%d" % i) for i in range(2)], "r10")
            rtm2 = Rot([Buf(arD.alloc([512], F32), "ru%d" % i) for i in range(2)], "r11")
            ssq_ps = Rot(PB[4:6], "m_ssq")
            rot_ps = Rot(PB[6:8], "m_rot")
            raw_ps = Rot(PB[0:4], "m_raw")
            R = slice(64, 96)
            TWO_PI = 2.0 * math.pi

            def sincos(out, phase):
                fw.tscalar("dve", tqb[R, :], angb[R, :], phase, 1.0 / TWO_PI, ALU.add, ALU.mult)
                fw.copy("dve", kib[R, :], tqb[R, :])
                fw.copy("dve", kfb[R, :], kib[R, :])
                fw.tscalar("dve", tqb[R, :], angb[R, :], phase, None, ALU.add)
                fw.stt("dve", tqb[R, :], kfb[R, :], -TWO_PI, tqb[R, :], ALU.mult, ALU.add)
                fw.tscalar("dve", kfb[R, :], tqb[R, :], math.pi, TWO_PI, ALU.is_gt, ALU.mult)
                fw.tt("dve", tqb[R, :], tqb[R, :], kfb[R, :], ALU.subtract)
                fw.tscalar("dve", kfb[R, :], tqb[R, :], -math.pi, TWO_PI, ALU.is_lt, ALU.mult)
                fw.tt("dve", tqb[R, :], tqb[R, :], kfb[R, :], ALU.add)
                fw.act(out[R, :], tqb[R, :], AF.Sin)

            tabs = {}

            def rope_item(tg):
                cols = slice(tg * 512, (tg + 1) * 512)
                load("sp", posi[0:96, :], pos_d[s:s + 1, cols].partition_broadcast(96))
                fw.copy("dve", angb[R, :], posi[R, :])
                fw.tscalar("dve", angb[R, :], angb[R, :], cA[R, C_FRQ:C_FRQ + 1], None, ALU.mult)
                yield
                S_ = ropeS_r.next()
                C_ = ropeC_r.next()
                sincos(S_, 0.0)
                yield
                sincos(C_, math.pi / 2)
                tabs[tg] = (C_, S_)

            def normrope_item(tg, mk_raw, gcol, out):
                rp = mk_raw()
                yield
                yield from norm_gen([rp[0:96, :]], 96, ones96, 1, 1.0, [gcol], [out], ssq_ps, sqr, sfr)
                raw_ps.release(rp)
                yield
                rp2 = rot_ps.next()
                fw.matmul(rp2[0:96, :], PTm, out)
                yield
                C_, S_ = tabs[tg]
                t1 = rtmp.next()
                t2 = rtm2.next()
                fw.tt("dve", t1[R, :], rp2[R, :], S_[R, :], ALU.mult)
                rot_ps.release(rp2)
                fw.tt("pool", t2[R, :], out[R, :], C_[R, :], ALU.mult)
                yield
                fw.tt("pool", out[R, :], t2[R, :], t1[R, :], ALU.add)

            cur = {}

            def ckv_item(tg):
                cols = slice(tg * 512, (tg + 1) * 512)
                raws = []
                for c in range(2):
                    rp = raw_ps.next()
                    for k in range(8):
                        fw.matmul(rp.v(), wc[:, k, c * 128:(c + 1) * 128], hT[:, k, cols], start=(k == 0), stop=(k == 7), signal=(k == 7))
                    raws.append(rp.v())
                ck = ckvn_r.next()
                ckvn_r.release(ck)
                cur[("ckvn", tg)] = ck
                yield
                yield from norm_gen(raws, 128, ones128, 0, 1.0 / 256, [cA[:, C_GC:C_GC + 1], cA[:, C_GC + 1:C_GC + 2]],
                                    [ck[:, 0, :], ck[:, 1, :]], ssq_ps, sqr, sfr)
                for r_ in raws:
                    raw_ps.release(r_.buf)

            def kr_item(tg):
                cols = slice(tg * 512, (tg + 1) * 512)
                rp = raw_ps.next()
                for k in range(8):
                    fw.matmul(rp[0:96, :], Wkr[:, k, :], hT[:, k, cols], start=(k == 0), stop=(k == 7), signal=(k == 7))
                kr = krs_r.next()
                krs_r.release(kr)
                cur[("krs", tg)] = kr
                yield
                fw.copy("act", kr[0:96, :], rp[0:96, :])

            def mk_k(tg, h):
                def f():
                    ck = cur[("ckvn", tg)]
                    kr = cur[("krs", tg)]
                    rp = raw_ps.next()
                    for c in range(2):
                        fw.matmul(rp[0:96, :], wkk[:, c, h, :], ck[:, c, :], start=(c == 0), stop=False, signal=False)
                    fw.matmul(rp[0:96, :], ISH, kr[0:96, :], start=False, stop=True)
                    return rp
                return f

            def mk_q(tg, h):
                def f():
                    cols = slice(tg * 512, (tg + 1) * 512)
                    rp = raw_ps.next()
                    for k in range(8):
                        fw.matmul(rp[0:96, :], wq[:, k, h * 96:(h + 1) * 96], hT[:, k, cols], start=(k == 0), stop=(k == 7), signal=(k == 7))
                    return rp
                return f

            def v_item(tg, t4):
                ck = cur[("ckvn", tg)]
                rp = raw_ps.next()
                for c in range(2):
                    fw.matmul(rp.v(), ck[:, c, t4 * 128:(t4 + 1) * 128], wkv[:, c, :, :].re("p h d -> p (h d)"),
                              start=(c == 0), stop=(c == 1), signal=(c == 1))
                yield
                fw.copy("act", vaug[:, tg * 4 + t4, :, 0:64], rp.v().re("p (h d) -> p h d", h=8))

            def mla_prep_items():
                yield rope_item(0)
                for tg in range(4):
                    cols = slice(tg * 512, (tg + 1) * 512)
                    yield ckv_item(tg)
                    yield kr_item(tg)
                    for h in range(4):
                        yield normrope_item(tg, mk_q(tg, h), gsc[0:96, 0:1], qT[h][0:96, cols])
                    if tg + 1 < 4:
                        yield rope_item(tg + 1)
                    for h in range(8):
                        yield normrope_item(tg, mk_k(tg, h), gsc[0:96, 1:2], kT[h][0:96, cols])
                        if h + 4 < 8:
                            yield normrope_item(tg, mk_q(tg, h + 4), gsc[0:96, 0:1], qT[h + 4][0:96, cols])
                    for t4 in range(4):
                        yield v_item(tg, t4)
            pipeline(mla_prep_items(), 3)
            if stop == 1:
                fw.finish()
                return nc, dbg_out

            arD.reset(markD)
            er = Rot([Buf(arD.alloc([512], BF16), "e%d" % i) for i in range(7)], "r12")
            utr = Rot([Buf(arD.alloc([512], F32), "ut%d" % i) for i in range(2)], "r13")
            rdr = Rot([Buf(arD.alloc([512], F32), "rd%d" % i) for i in range(2)], "r14")
            s_ps = Rot(PB[0:5], "a_s")
            u_ps = Rot(PB[5:7], "a_u")
            b_ps = Rot(PB[7:8], "a_b")
            scale_m = 96.0 ** -0.5
            ubank = {}

            def mla_item(h, j, kt):
                nq0 = max(0, kt - 4 * j) * 128
                N = 512 - nq0
                sp_ = s_ps.next()
                fw.matmul(sp_[:, 0:N], kT[h][0:96, kt * 128:(kt + 1) * 128], qT[h][0:96, j * 512 + nq0:(j + 1) * 512])
                yield
                e = er.next()
                fw.act(e[:, 0:N], sp_[:, 0:N], AF.Exp, scale=scale_m)
                s_ps.release(sp_)
                yield
                if kt >= 4 * j:
                    fw.tt("pool", e[:, 0:128], e[:, 0:128], caus, ALU.mult)
                    yield
                if kt == 0:
                    ubank[(h, j)] = u_ps.next()
                up = ubank[(h, j)]
                last = (kt == 4 * j + 3)
                fw.matmul(up[0:65, nq0:512], vaug[:, kt, h, :], e[:, 0:N], start=(kt == 0), stop=last, signal=last)
                er.release(e)
                if last:
                    yield
                    ut = utr.next()
                    fw.copy("act", ut[0:65, :], up[0:65, :])
                    yield
                    bp = b_ps.next()
                    fw.matmul(bp[0:64, :], Ef.v(), ut[0:65, :])
                    yield
                    rd = rdr.next()
                    fw.act(rd[0:64, :], bp[0:64, :], AF.Ln)
                    yield
                    fw.act(rd[0:64, :], rd[0:64, :], AF.Exp, scale=-1.0)
                    yield
                    fw.tt("dve", omla[0:64, h, j * 512:(j + 1) * 512], ut[0:64, :], rd[0:64, :], ALU.mult)
            pipeline((mla_item(h, j, kt) for h in range(8) for j in range(4) for kt in range(4 * j + 4)), 5)
            if stop == 2:
                fw.finish()
                return nc, dbg_out

            LAST_ALLOC[0] = (C_OFF, 32768)
            odil = Buf(shaped(arena_t[:, C_OFF:C_OFF + 32768].bitcast(BF16), [8, S]), "odil")
            for hh in range(2):
                arD.reset()
                Ut = [Buf(arD.alloc([S], F32), "Ut") for _ in range(4)]
                wqg = Buf(arD.alloc([8, 256], BF16), "wqg")
                wkg = Buf(arD.alloc([8, 256], BF16), "wkg")
                wvg = Buf(arD.alloc([8, 256], BF16), "wvg")
                mtbs = [Buf(arD.alloc([4, 256], BF16), "mtb%d" % i) for i in range(2)]
                markU = arD.off

                def dil_loads(g_):
                    for which, dst in enumerate((wqg, wkg, wvg)):
                        slab_load(dst.v(), DILB + which * 1536 + g_ * 512 + hh * 256, 256)
                    load("pool", mtbs[g_ % 2].v(), mt_d[g_, :, hh * 4:hh * 4 + 4, :])
                dil_loads(0)
                qg = Buf(arD.alloc([2, S], BF16), "qg")
                kg = Buf(arD.alloc([2, S], BF16), "kg")
                vg = Buf(arD.alloc([NT, 4, 65], BF16), "vg")
                fw.memset("pool", vg[:, :, :, 64:65], 1.0)
                sqr = Rot([Buf(arD.alloc([512], BF16), "sq%d" % i) for i in range(4)], "r15")
                sfr = Rot([Buf(arD.alloc([512], F32), "sf%d" % i) for i in range(3)], "r16")
                er = Rot([Buf(arD.alloc([512], BF16), "e%d" % i) for i in range(7)], "r17")
                for g in range(3):
                    dil = DILS[g]
                    nb = NBLK[g]
                    mtb = mtbs[g % 2]
                    ssq_ps = Rot(PB[4:6], "d_ssq")
                    raw_ps = Rot(PB[0:4] + PB[6:8], "d_raw")

                    def hview(k, j0, nt):
                        if g == 0:
                            return hT[:, k, j0 * 128:(j0 + nt) * 128]
                        if g == 1:
                            r, b = j0 // 4, j0 % 4
                            st0 = r + 512 * b
                            return hT[:, k, st0:st0 + 512 * nt - 3:4] if nt < 4 else hT[:, k, r:S:4]
                        if nt == 1:
                            return hT[:, k, j0:S:16]
                        return View(hT, hT.ap[:, k, :].rearrange("p (i s) -> p s i", s=16)[:, j0:j0 + nt, :])

                    def qk_item(tg, wsl, gi, dstb, c):
                        cols = slice(tg * 512, (tg + 1) * 512)
                        rp = raw_ps.next()
                        for k in range(8):
                            fw.matmul(rp.v(), wsl[:, k, c * 128:(c + 1) * 128], hT[:, k, cols],
                                      start=(k == 0), stop=(k == 7), signal=(k == 7))
                        yield
                        if g == 0:
                            outv, xf = dstb[:, c, cols], None
                        else:
                            rr = dil
                            un = 512 // rr
                            outv = dstb[:, c, :].re("p (r u) -> p r u", r=rr)[:, :, un * tg:un * (tg + 1)]
                            xf = (lambda v, rr=rr: v.re("p (u r) -> p r u", r=rr))
                        yield from norm_gen([rp.v()], 128, onesblk, 2, 1.0, [gsc[:, gi:gi + 1]], [outv], ssq_ps, sqr, sfr, xf=xf)
                        raw_ps.release(rp)

                    def vd_item(jp):
                        rp = raw_ps.next()
                        for k in range(8):
                            fw.matmul(rp[:, 0:256], hview(k, jp, 1), wvg[:, k, :], start=(k == 0), stop=(k == 7), signal=(k == 7))
                        yield
                        fw.copy("act", vg[:, jp, :, 0:64], rp[:, 0:256].re("p (h d) -> p h d", h=4))

                    def dprep_items():
                        for tg in range(4):
                            for (wsl, gi, dstb) in ((wqg, 2 + g, qg), (wkg, 5 + g, kg)):
                                for c in range(2):
                                    yield qk_item(tg, wsl, gi, dstb, c)
                            for t4 in range(4):
                                yield vd_item(tg * 4 + t4)
                    pipeline(dprep_items(), 5)
                    if g + 1 < 3:
                        dil_loads(g + 1)

                    s_ps = Rot(PB[0:5])
                    ubanks = Rot(PB[5:8])
                    bank = {}

                    def evac_bank(hl, n):
                        up = bank.pop((hl, n))
                        src = up[0:65, :]
                        if g == 0:
                            fw.copy("act", Ut[hl][0:65, n * 512:(n + 1) * 512], src)
                            return
                        if g == 1:
                            dst = Ut[hl][0:65, n:S:4]
                        else:
                            dst = View(Ut[hl], Ut[hl].ap[0:65, :].rearrange("p (i s) -> p s i", s=16)[:, 4 * n:4 * n + 4, :])
                            src = src.re("p (t i) -> p t i", t=4)
                        fw.tt("dve", dst, dst, src, ALU.add)

                    def dil_item(hl, pr):
                        c, r0 = hl // 2, (hl % 2) * 64
                        rows = slice(r0, r0 + 64)
                        tiles = []
                        off = 0
                        sp_ = s_ps.next()
                        for jp in (2 * pr, 2 * pr + 1):
                            b = jp % nb
                            N = 256 if b + 1 < nb else 128
                            tiles.append((jp, off, N))
                            fw.matmul(sp_[:, off:off + N], kg[rows, c, jp * 128:(jp + 1) * 128], qg[rows, c, jp * 128:jp * 128 + N])
                            off += N
                        yield
                        e = er.next()
                        fw.act(e[:, 0:off], sp_[:, 0:off], AF.Exp, scale=0.125)
                        s_ps.release(sp_)
                        yield
                        for (jp, o, N) in tiles:
                            fw.tt("dve", e[:, o:o + N], e[:, o:o + N], mtb[:, hl, 0:N], ALU.mult)
                        yield
                        for (jp, o, N) in tiles:
                            targets = [(jp, e[:, o:o + 128])]
                            if N == 256:
                                targets.append((jp + 1, e[:, o + 128:o + 256]))
                            for (tj, rhs) in targets:
                                n = tj // 4
                                first = (hl, n) not in bank
                                if first:
                                    bank[(hl, n)] = ubanks.next()
                                up = bank[(hl, n)]
                                fw.matmul(up[0:65, (tj % 4) * 128:(tj % 4) * 128 + 128], vg[:, jp, hl, :], rhs,
                                          start=first, stop=True, signal=True, skip_group_check=True)
                            if jp % 4 == 3:
                                evac_bank(hl, jp // 4)
                    pipeline((dil_item(hl, pr) for hl in range(4) for pr in range(8)), 5)
                arD.reset(markU)
                utb = Rot([Buf(arD.alloc([512], F32), "rd%d" % i) for i in range(3)], "r18")
                b_ps = Rot(PB[0:4])

                def dn_item(hl, j):
                    cols = slice(j * 512, (j + 1) * 512)
                    bp = b_ps.next()
                    fw.matmul(bp[0:64, :], Ef.v(), Ut[hl][0:65, cols])
                    yield
                    rd = utb.next()
                    fw.act(rd[0:64, :], bp[0:64, :], AF.Ln)
                    yield
                    fw.act(rd[0:64, :], rd[0:64, :], AF.Exp, scale=-1.0)
                    yield
                    fw.tt("dve", odil[0:64, hh * 4 + hl, cols], Ut[hl][0:64, cols], rd[0:64, :], ALU.mult)
                pipeline((dn_item(hl, j) for hl in range(4) for j in range(4)), 3)
                if stop == 3:
                    fw.finish()
                    return nc, dbg_out

            arD.reset()
            mrg = Buf(arD.alloc([8, S], BF16), "mrg")
            mark_mrg = arD.off
            woa = Buf(arD.alloc([8, DM], BF16), "woa")
            wob = Buf(arD.alloc([8, DM], BF16), "wob")
            load("pool", woa[0:64, :, :], woa_d.rearrange("(h d) n -> d h n", d=64))
            load("pool", wob[0:64, :, :], wob_d.rearrange("(h d) n -> d h n", d=64))
            wgs = Rot([Buf(arD.alloc([8, 256], BF16), "wgs%d" % i) for i in range(2)], "r19")
            sgr = Rot([Buf(arD.alloc([512], F32), "sg%d" % i) for i in range(4)], "r20")
            m1r = Rot([Buf(arD.alloc([512], F32), "m1%d" % i) for i in range(5)], "r21")
            ps4 = Rot(PB[0:8])
            wgcur = {}

            m1A = {}

            def wg_load(c_):
                wgc_ = wgs.next()
                wgs.release(wgc_)
                slab_load(wgc_[:, :, 0:128], GATEB + c_ * 128, 128)
                slab_load(wgc_[:, :, 128:256], GATEB + 1024 + c_ * 128, 128)
                wgcur[c_] = wgc_
            wg_load(0)

            def mg_item(c, tg, br):
                if tg == 0 and br == 0 and c + 1 < 8:
                    wg_load(c + 1)
                wgc = wgcur[c]
                cols = slice(tg * 512, (tg + 1) * 512)
                wo, osrc = ((woa, omla), (wob, odil))[br]
                gp = ps4.next()
                for k in range(8):
                    fw.matmul(gp.v(), wgc[:, k, br * 128:(br + 1) * 128], hT[:, k, cols], start=(k == 0), stop=(k == 7), signal=(k == 7))
                pp = ps4.next()
                for h in range(8):
                    fw.matmul(pp.v(), wo[0:64, h, c * 128:(c + 1) * 128], osrc[0:64, h, cols], start=(h == 0), stop=(h == 7), signal=(h == 7))
                yield
                sg = sgr.next()
                fw.act(sg.v(), gp.v(), AF.Sigmoid, bias=cA[:, C_BG + br * 8 + c:C_BG + br * 8 + c + 1])
                ps4.release(gp)
                yield
                m1 = m1r.next()
                fw.tt("dve", m1.v(), sg.v(), pp.v(), ALU.mult)
                ps4.release(pp)
                sgr.release(sg)
                if br == 0:
                    m1r.hold(m1)
                    m1A[(c, tg)] = m1
                    return
                yield
                ma = m1A.pop((c, tg))
                fw.tt("pool", mrg[:, c, cols], ma.v(), m1.v(), ALU.add)
                m1r.release(ma)

            pipeline((mg_item(c, tg, br) for c in range(8) for tg in range(4) for br in range(2)), 4)
            fw_barrier(fw)
            if stop == 4:
                fw.finish()
                return nc, dbg_out

            arAB.reset()
            arC.reset()
            x1 = [Buf(arAB.alloc([DM], F32), "x1") for _ in range(NT)]
            h2T = Buf(arC.alloc([8, S], BF16), "h2T")
            arD.reset(mark_mrg)
            wo_ = Buf(arD.alloc([8, DM], BF16), "wout")
            load("pool", wo_.v(), wout_d.rearrange("(c p) n -> p c n", p=128))
            junk = Buf(arD.alloc([DM], BF16), "junk")
            xbs = Rot([Buf(arD.alloc([DM], BF16), "xb%d" % i) for i in range(3)], "r22")
            sts = Rot([Buf(arD.alloc([4], F32), "st%d" % i) for i in range(4)], "r23")
            xts = Rot([Buf(arD.alloc([DM], F32), "xt%d" % i) for i in range(3)], "r24")
            y_ps = Rot(PW[0:3], "p3y")
            t_ps = Rot(PB[6:7], "p3t")
            l_ps = Rot(PB[7:8], "p3l")
            gF_bc = View(cA, cA.ap[:, C_GF:C_GF + 8].unsqueeze(2).broadcast_to([128, 8, 128]))

            def p3_item(t):
                tc_ = slice(t * 128, (t + 1) * 128)
                xt = xts.next()
                load("sp", xt.v(), x_d[s, tc_, :])
                yp = y_ps.next()
                for half in range(2):
                    for k in range(8):
                        fw.matmul(yp[:, half * 512:(half + 1) * 512], mrg[:, k, tc_], wo_[:, k, half * 512:(half + 1) * 512],
                                  start=(k == 0), stop=(k == 7), signal=(k == 7 and half == 1))
                yield
                fw.tt("dve", x1[t].v(), yp.v(), xt.v(), ALU.add)
                y_ps.release(yp)
                yield
                stt_ = sts.next()
                fw.act(junk.v(), x1[t].v(), AF.Square, accum_out=stt_[:, 0:1])
                yield
                fw.act(stt_[:, 1:2], stt_[:, 0:1], AF.Sqrt, scale=1.0 / DM, bias=epsc[:, 0:1])
                yield
                fw.recip(stt_[:, 2:3], stt_[:, 1:2])
                yield
                xb = xbs.next()
                fw.tscalar("dve", xb.v(), x1[t].v(), stt_[:, 2:3], None, ALU.mult)
                yield
                pb = t_ps.next()
                pbv = View(pb, shaped(pb.ap.bitcast(BF16), [8, 128]))
                for k in range(8):
                    fw.transpose(pbv[:, k, :], xb[:, k * 128:(k + 1) * 128], ident, signal=(k == 7))
                yield
                fw.tt("dve", h2T[:, :, tc_], pbv, gF_bc, ALU.mult)
                t_ps.release(pb)
                yield
                lp = l_ps.next()
                for k in range(8):
                    fw.matmul(lp[:, 0:36], h2T[:, k, tc_], wrb[:, k, :], start=(k == 0), stop=(k == 7), signal=(k == 7))
                yield
                fw.tt("dve", LG[:, t, :], lp[:, 0:36], cA[:, C_BR:C_BR + 36], ALU.add)
            pipeline((p3_item(t) for t in range(NT)), 3)

            def bc3(v, n):
                return View(v.buf, v.ap.unsqueeze(2).broadcast_to([128, NT, n]))
            gmx, gsum, gp_, m1_, m2_, dd, w1, w2 = [RS[:, :, i] for i in range(8)]
            ohg = RS[:, :, 8:12]
            eg = RS[:, :, 12:16]
            els = RS[:, :, 16:24]
            oh1 = RS[:, :, 24:32]
            msk = RS[:, :, 32:40]
            oh2 = RS[:, :, 40:48]
            tmp8 = RS[:, :, 48:56]
            fw.reduce("dve", gmx, LG[:, :, 0:4], ALU.max)
            fw.tt("dve", ohg, LG[:, :, 0:4], bc3(gmx, 4), ALU.is_equal)
            fw.tt("dve", eg, LG[:, :, 0:4], bc3(gmx, 4), ALU.subtract)
            fw.act(eg, eg, AF.Exp)
            fw.reduce("dve", gsum, eg, ALU.add)
            fw.recip(gp_, gsum)
            for gi in range(4):
                dstv = els if gi == 0 else tmp8
                fw.tt("dve", dstv, LG[:, :, 4 + gi * 8:12 + gi * 8], bc3(ohg[:, :, gi], 8), ALU.mult)
                if gi > 0:
                    fw.tt("dve", els, els, tmp8, ALU.add)
            fw.reduce("dve", m1_, els, ALU.max)
            fw.tt("dve", oh1, els, bc3(m1_, 8), ALU.is_equal)
            fw.stt("dve", msk, oh1, -1e30, els, ALU.mult, ALU.add)
            fw.reduce("dve", m2_, msk, ALU.max)
            fw.tt("dve", oh2, msk, bc3(m2_, 8), ALU.is_equal)
            fw.tt("dve", dd, m2_, m1_, ALU.subtract)
            fw.act(dd, dd, AF.Exp)
            fw.tscalar("dve", w2, dd, 1.0, None, ALU.add)
            fw.recip(w2, w2)
            fw.tt("dve", w1, w2, gp_, ALU.mult)
            fw.tt("dve", w2, w1, dd, ALU.mult)
            fw.tt("dve", oh1, oh1, bc3(w1, 8), ALU.mult)
            fw.tt("dve", oh2, oh2, bc3(w2, 8), ALU.mult)
            fw.tt("dve", oh1, oh1, oh2, ALU.add)
            for gi in range(4):
                fw.tt("dve", comb[:, :, gi * 8:(gi + 1) * 8], oh1, bc3(ohg[:, :, gi], 8), ALU.mult)
            fw_barrier(fw)
            if stop == 5:
                fw.finish()
                return nc, dbg_out

            arD.reset()
            slots = Rot([Buf(arD.alloc([3, 8, 256], BF16), "ex%d" % i) for i in range(3)], "r25")
            sgr = Rot([Buf(arD.alloc([512], F32), "sg%d" % i) for i in range(2)], "r26")
            her = Rot([Buf(arD.alloc([2, 512], BF16), "he%d" % i) for i in range(3)], "r27")
            gu_ps = Rot(PB[0:4])
            y_ps = Rot(PW[2:4])

            def ex_load(e):
                sl = slots.next()
                load("pool", sl[:, 0, :, :], wg_d[e].rearrange("(c p) n -> p c n", p=128))
                load("pool", sl[:, 1, :, :], wu_d[e].rearrange("(c p) n -> p c n", p=128))
                load("pool", sl[:, 2, :, :].re("p (c x) n -> p c (x n)", c=2), wd_d[e].rearrange("(c p) n -> p c n", p=128))
                return sl
            exq = [ex_load(0), ex_load(1)]
            msteps = [(e, tg) for e in range(32) for tg in range(4)]
            mstate = {}

            def moe_front_half(st_, ffc):
                e, tg = st_
                if tg == 1 and ffc == 0:
                    if e + 2 < 32:
                        exq.append(ex_load(e + 2))
                sl = exq[e]
                cols = slice(tg * 512, (tg + 1) * 512)
                if ffc == 0:
                    mstate[st_] = her.next()
                he = mstate[st_]
                gp = gu_ps.next()
                up = gu_ps.next()
                for k in range(8):
                    fw.matmul(gp.v(), sl[:, 0, k, ffc * 128:(ffc + 1) * 128], h2T[:, k, cols], start=(k == 0), stop=(k == 7), signal=(k == 7))
                for k in range(8):
                    fw.matmul(up.v(), sl[:, 1, k, ffc * 128:(ffc + 1) * 128], h2T[:, k, cols], start=(k == 0), stop=(k == 7), signal=(k == 7))
                sg = sgr.next()
                fw.act(sg.v(), gp.v(), AF.Silu)
                fw.tt("dve", he[:, ffc, :], sg.v(), up.v(), ALU.mult)

            def moe_back_half(st_, hf):
                e, tg = st_
                sl = exq[e]
                he = mstate[st_]
                wdv = sl[:, 2, :, :].re("p (c x) n -> p c (x n)", c=2)
                for t4 in (2 * hf, 2 * hf + 1):
                    t = tg * 4 + t4
                    yp = y_ps.next()
                    for half in range(2):
                        for ffc in range(2):
                            fw.matmul(yp[:, half * 512:(half + 1) * 512], he[:, ffc, t4 * 128:(t4 + 1) * 128],
                                      wdv[:, ffc, half * 512:(half + 1) * 512], start=(ffc == 0), stop=(ffc == 1),
                                      signal=(ffc == 1 and half == 1))
                    fw.stt("dve", x1[t].v(), yp.v(), comb[:, t, e:e + 1], x1[t].v(), ALU.mult, ALU.add)
                    if e == 31:
                        fw.dma("sp", y_d[s, t * 128:(t + 1) * 128, :], x1[t].v(), out_is_dram=True, sem=ssem)
                if hf == 1:
                    mstate.pop(st_)

            for i in range(len(msteps) + 1):
                for hf in range(2):
                    if i < len(msteps):
                        moe_front_half(msteps[i], hf)
                    if i - 1 >= 0:
                        moe_back_half(msteps[i - 1], hf)
            fw_barrier(fw)
        fw.finish()
    return nc, dbg_out


def _consts():
    cm = np.zeros((128, NCM), np.float32)
    cm[:, M_ID:M_ID + 128] = np.eye(128, dtype=np.float32)
    cm[:, M_ONES:M_ONES + 128] = 1.0
    blk = np.zeros((128, 128), np.float32)
    blk[:64, :64] = 1.0
    blk[64:, 64:] = 1.0
    cm[:, M_BLK:M_BLK + 128] = blk
    cm[:96, M_O96:M_O96 + 96] = 1.0
    pt = np.zeros((96, 96), np.float32)
    for i in range(16):
        pt[80 + i, 64 + i] = -1.0
        pt[64 + i, 80 + i] = 1.0
    cm[:96, M_PT:M_PT + 96] = pt
    ish = np.zeros((96, 96), np.float32)
    for i in range(64, 96):
        ish[i, i] = 1.0
    cm[:96, M_ISH:M_ISH + 96] = ish
    p = np.arange(128)[:, None]
    f = np.arange(128)[None, :]
    cm[:, M_CAUS:M_CAUS + 128] = (f >= p).astype(np.float32)
    ce = np.zeros((65, 64), np.float32)
    ce[64, :] = 1.0
    mt = np.zeros((3, 128, 8, 256), np.float32)
    slopes = np.exp2(-8.0 * (np.arange(8, dtype=np.float32) + 1.0) / 8.0).astype(np.float32)
    for g, dil in enumerate(DILS):
        for h in range(8):
            dcur = (f - p).astype(np.float32)
            cur = np.where(f >= p, np.exp(-slopes[h] * dcur * dil), 0.0)
            dprev = (128 + f - p).astype(np.float32)
            prev = np.where(f <= p, np.exp(-slopes[h] * dprev * dil), 0.0)
            mt[g, :, h, 0:128] = cur
            mt[g, :, h, 128:256] = prev
    freq = (10000.0 ** (-np.arange(16, dtype=np.float32) / 16.0)).astype(np.float32)
    return cm, ce, mt, freq


def kernel(x, positions, norm_attn, w_in, b_gate, norm_ckv, w_ukv, q_norm_mla, k_norm_mla,
           q_norm_dil, k_norm_dil, w_o_mla, w_o_dil, w_out, norm_ffn, w_router_group,
           b_router_group, w_router_expert, b_router_expert, w_gate, w_up, w_down):
    f = lambda a: np.ascontiguousarray(np.asarray(a), dtype=np.float32)
    x = f(x)
    positions = np.ascontiguousarray(np.asarray(positions), dtype=np.int32)
    ncores = 8
    nseq = x.shape[0] // ncores
    cm, ce, mt, freq = _consts()
    ca = np.zeros((128, NCA), np.float32)
    ca[:, C_GA:C_GA + 8] = f(norm_attn)[0].reshape(8, 128).T
    ca[:, C_GF:C_GF + 8] = f(norm_ffn)[0].reshape(8, 128).T
    ca[:, C_GC:C_GC + 2] = f(norm_ckv)[0].reshape(2, 128).T
    ca[:, C_BG:C_BG + 16] = f(b_gate)[0].reshape(16, 128).T
    ca[:96, C_GQM] = f(q_norm_mla)[0]
    ca[:96, C_GKM] = f(k_norm_mla)[0]
    ca[:, C_GQD:C_GQD + 3] = np.tile(f(q_norm_dil)[0], (1, 2)).T
    ca[:, C_GKD:C_GKD + 3] = np.tile(f(k_norm_dil)[0], (1, 2)).T
    ca[64:80, C_FRQ] = freq
    ca[80:96, C_FRQ] = freq
    ca[:, C_BR:C_BR + 4] = f(b_router_group)[0][None, :]
    ca[:, C_BR + 4:C_BR + 36] = f(b_router_expert)[0][None, :]
    wr = np.ascontiguousarray(np.concatenate([f(w_router_group)[0], f(w_router_expert)[0]], axis=1))
    shared = {
        "w_in": f(w_in)[0], "w_ukv": f(w_ukv)[0], "w_o_mla": f(w_o_mla)[0], "w_o_dil": f(w_o_dil)[0],
        "w_out": f(w_out)[0], "w_gate": f(w_gate)[0], "w_up": f(w_up)[0], "w_down": f(w_down)[0],
        "wr": wr, "cst_a": ca, "cst_m": cm, "cst_e": ce, "mtab": mt,
    }
    nc, _ = build_nc(nseq)
    in_maps = []
    for c in range(ncores):
        m = dict(shared)
        m["x"] = np.ascontiguousarray(x[c * nseq:(c + 1) * nseq])
        m["pos"] = np.ascontiguousarray(positions[c * nseq:(c + 1) * nseq])
        in_maps.append(m)
    res = run_bass_kernel_spmd(nc, in_maps, core_ids=list(range(ncores)))
    out = np.concatenate([np.asarray(r["y"]) for r in res.results], axis=0)
    return out.astype(np.float32)
```
